# Optimizing a Trainium2 kernel written in Bass

```python
import jax, jax.numpy as jnp
from jax import lax
import numpy as np

D_MODEL = 1024
BATCH = 8
SEQ = 4096
DEPTH = 4
DEC_BATCH = 4
DEC_SEQ = 8192
PAST_LEN = 128

N_HEADS = 8
QK_NOPE_DIM = 64
QK_ROPE_DIM = 32
V_HEAD_DIM = 64
QK_DIM = QK_NOPE_DIM + QK_ROPE_DIM
Q_LORA_RANK = 384
KV_LORA_RANK = 256
ATTN_WIDTH = N_HEADS * V_HEAD_DIM
F_GROUPS = 8
F_GROUP_DIM = 64
F_WIDTH = F_GROUPS * F_GROUP_DIM
IN_WIDTH = Q_LORA_RANK + KV_LORA_RANK + QK_ROPE_DIM + F_WIDTH + 2 * D_MODEL
N_EXPERTS = 32
TOP_K = 4
D_FF = D_MODEL
SWIGLU_LIMIT = 7.0
SWIGLU_ALPHA = 1.702
ROPE_THETA = 10000.0
RMS_EPS = 1e-6
Q_BLOCK = 128
ROW_BLOCK = 256
N_MOD = 6

kernel_name = 'hybrid_mla_fnet_moe_adaln_encoder'


def rmsnorm(x, g):
    xf = x.astype(jnp.float32)
    xf = xf * lax.rsqrt(jnp.mean(xf * xf, axis=-1, keepdims=True) + RMS_EPS)
    return xf.astype(x.dtype) * g


def rope_tables(seq, dtype):
    inv = 1.0 / (ROPE_THETA ** (jnp.arange(0, QK_ROPE_DIM, 2, dtype=jnp.float32) / QK_ROPE_DIM))
    ang = jnp.arange(seq, dtype=jnp.float32)[:, None] * inv[None, :]
    return jnp.cos(ang).astype(dtype), jnp.sin(ang).astype(dtype)


def apply_rope(t, cos, sin):
    half = QK_ROPE_DIM // 2
    t1, t2 = t[..., :half], t[..., half:]
    c = cos[None, :, None, :]
    s = sin[None, :, None, :]
    return jnp.concatenate([t1 * c - t2 * s, t1 * s + t2 * c], axis=-1)


def mla_branch(u_q, u_kv, u_kr, g_q, w_uq, g_kv, w_ukv):
    bsz, seq, _ = u_q.shape
    cos, sin = rope_tables(seq, u_q.dtype)
    q = (rmsnorm(u_q, g_q) @ w_uq).reshape(bsz, seq, N_HEADS, QK_DIM)
    q = jnp.concatenate([q[..., :QK_NOPE_DIM], apply_rope(q[..., QK_NOPE_DIM:], cos, sin)], axis=-1)
    q = q * (QK_DIM ** -0.5)
    kv = (rmsnorm(u_kv, g_kv) @ w_ukv).reshape(bsz, seq, N_HEADS, QK_NOPE_DIM + V_HEAD_DIM)
    k_nope, v = kv[..., :QK_NOPE_DIM], kv[..., QK_NOPE_DIM:]
    k_rope = apply_rope(u_kr[:, :, None, :], cos, sin)
    k = jnp.concatenate([k_nope, jnp.broadcast_to(k_rope, (bsz, seq, N_HEADS, QK_ROPE_DIM))], axis=-1)
    n_blk = seq // Q_BLOCK
    qb = q.reshape(bsz, n_blk, Q_BLOCK, N_HEADS, QK_DIM).transpose(1, 0, 2, 3, 4)

    def attend(q_blk):
        s = jnp.einsum('bqhd,bkhd->bhqk', q_blk, k).astype(jnp.float32)
        p = jax.nn.softmax(s, axis=-1).astype(v.dtype)
        return jnp.einsum('bhqk,bkhd->bqhd', p, v)

    o = lax.map(attend, qb)
    return o.transpose(1, 0, 2, 3, 4).reshape(bsz, seq, ATTN_WIDTH)


def fourier_branch(u_f):
    bsz, seq, _ = u_f.shape
    z = u_f.astype(jnp.float32).reshape(bsz, seq, F_GROUPS, F_GROUP_DIM)
    y = jnp.fft.fft2(z, axes=(1, 3), norm='ortho').real
    return y.reshape(bsz, seq, F_WIDTH).astype(u_f.dtype)


def moe(h, w_router, b_router, w_gu, b_gu, w_dn, b_dn):
    bsz, seq, d = h.shape
    t = bsz * seq
    hf = h.reshape(t, d)
    logits = (hf @ w_router + b_router).astype(jnp.float32)
    top_val, top_idx = lax.top_k(logits, TOP_K)
    gates = jax.nn.softmax(top_val, axis=-1)
    a = t * TOP_K
    e_flat = top_idx.reshape(a).astype(jnp.int32)
    tok_flat = jnp.arange(a, dtype=jnp.int32) // TOP_K
    g_flat = gates.reshape(a)
    order = jnp.argsort(e_flat)
    e_sorted = e_flat[order]
    counts = jnp.zeros((N_EXPERTS,), jnp.int32).at[e_flat].add(1)
    padded = (counts + ROW_BLOCK - 1) // ROW_BLOCK * ROW_BLOCK
    start = jnp.cumsum(counts) - counts
    pend = jnp.cumsum(padded)
    pstart = pend - padded
    dest = pstart[e_sorted] + (jnp.arange(a, dtype=jnp.int32) - start[e_sorted])
    n_blocks = (a + N_EXPERTS * ROW_BLOCK + ROW_BLOCK - 1) // ROW_BLOCK
    p = n_blocks * ROW_BLOCK
    row_tok = jnp.full((p,), t, jnp.int32).at[dest].set(tok_flat[order])
    row_gate = jnp.zeros((p,), jnp.float32).at[dest].set(g_flat[order])
    blk_start = jnp.arange(n_blocks, dtype=jnp.int32) * ROW_BLOCK
    blk_expert = jnp.minimum(jnp.searchsorted(pend, blk_start, side='right'), N_EXPERTS - 1).astype(jnp.int32)
    xs = jnp.concatenate([hf, jnp.zeros((1, d), hf.dtype)], axis=0)[row_tok].reshape(n_blocks, ROW_BLOCK, d)

    def expert_block(args):
        xb, e = args
        gu = xb @ w_gu[e] + b_gu[e]
        gate = jnp.minimum(gu[..., 0::2], SWIGLU_LIMIT)
        up = jnp.clip(gu[..., 1::2], -SWIGLU_LIMIT, SWIGLU_LIMIT)
        glu = gate * jax.nn.sigmoid(gate * SWIGLU_ALPHA)
        return ((up + 1.0) * glu) @ w_dn[e] + b_dn[e]

    ys = lax.map(expert_block, (xs, blk_expert)).reshape(p, d)
    ys = ys * row_gate[:, None].astype(ys.dtype)
    y = jnp.zeros((t + 1, d), ys.dtype).at[row_tok].add(ys)
    return y[:t].reshape(bsz, seq, d)


def encoder_layer(x, c, w_ada, b_ada, g_mix, g_ffn, w_in, g_q, w_uq, g_kv, w_ukv,
                  w_a, w_b, w_out, w_router, b_router, w_gu, b_gu, w_dn, b_dn):
    mod = jax.nn.silu(c) @ w_ada + b_ada
    sh1, sc1, ga1, sh2, sc2, ga2 = [m[:, None, :] for m in jnp.split(mod, N_MOD, axis=-1)]
    h = rmsnorm(x, g_mix) * (1.0 + sc1) + sh1
    u = h @ w_in
    o1 = Q_LORA_RANK
    o2 = o1 + KV_LORA_RANK
    o3 = o2 + QK_ROPE_DIM
    o4 = o3 + F_WIDTH
    o5 = o4 + D_MODEL
    y_a = mla_branch(u[..., :o1], u[..., o1:o2], u[..., o2:o3], g_q, w_uq, g_kv, w_ukv) @ w_a
    y_b = fourier_branch(u[..., o3:o4]) @ w_b
    merged = jax.nn.sigmoid(u[..., o4:o5]) * y_a + jax.nn.sigmoid(u[..., o5:]) * y_b
    x = x + ga1 * (merged @ w_out)
    h = rmsnorm(x, g_ffn) * (1.0 + sc2) + sh2
    return x + ga2 * moe(h, w_router, b_router, w_gu, b_gu, w_dn, b_dn)


def trunk(x, c, w_ada, b_ada, g_mix, g_ffn, w_in, g_q, w_uq, g_kv, w_ukv,
          w_a, w_b, w_out, w_router, b_router, w_gu, b_gu, w_dn, b_dn, g_final):
    for l in range(DEPTH):
        x = encoder_layer(x, c, w_ada[l], b_ada[l], g_mix[l], g_ffn[l], w_in[l], g_q[l], w_uq[l],
                          g_kv[l], w_ukv[l], w_a[l], w_b[l], w_out[l], w_router[l], b_router[l],
                          w_gu[l], b_gu[l], w_dn[l], b_dn[l])
    return rmsnorm(x, g_final)


def setup_inputs(seed: int = 0) -> dict:
    key = jax.random.key(seed)
    ks = jax.random.split(key, 24)
    f32 = jnp.float32

    def nrm(k, shape, scale):
        return jax.random.normal(k, shape, f32) * scale

    def gain(k, shape):
        return 1.0 + 0.02 * jax.random.normal(k, shape, f32)

    L, D = DEPTH, D_MODEL
    return {
        'x_prompt': nrm(ks[0], (BATCH, SEQ, D), 1.0),
        'x_sample': nrm(ks[1], (DEC_BATCH, DEC_SEQ, D), 1.0),
        'c_prompt': nrm(ks[2], (BATCH, D), 1.0),
        'c_sample': nrm(ks[3], (DEC_BATCH, D), 1.0),
        'w_ada': nrm(ks[4], (L, D, N_MOD * D), 0.5 * D ** -0.5),
        'b_ada': nrm(ks[5], (L, N_MOD * D), 0.02),
        'g_mix': gain(ks[6], (L, D)),
        'g_ffn': gain(ks[7], (L, D)),
        'w_in': nrm(ks[8], (L, D, IN_WIDTH), D ** -0.5),
        'g_q': gain(ks[9], (L, Q_LORA_RANK)),
        'w_uq': nrm(ks[10], (L, Q_LORA_RANK, N_HEADS * QK_DIM), Q_LORA_RANK ** -0.5),
        'g_kv': gain(ks[11], (L, KV_LORA_RANK)),
        'w_ukv': nrm(ks[12], (L, KV_LORA_RANK, N_HEADS * (QK_NOPE_DIM + V_HEAD_DIM)), KV_LORA_RANK ** -0.5),
        'w_a': nrm(ks[13], (L, ATTN_WIDTH, D), ATTN_WIDTH ** -0.5),
        'w_b': nrm(ks[14], (L, F_WIDTH, D), F_WIDTH ** -0.5),
        'w_out': nrm(ks[15], (L, D, D), D ** -0.5),
        'w_router': nrm(ks[16], (L, D, N_EXPERTS), D ** -0.5),
        'b_router': nrm(ks[17], (L, N_EXPERTS), 0.01),
        'w_gu': nrm(ks[18], (L, N_EXPERTS, D, 2 * D_FF), D ** -0.5),
        'b_gu': nrm(ks[19], (L, N_EXPERTS, 2 * D_FF), 0.02),
        'w_dn': nrm(ks[20], (L, N_EXPERTS, D_FF, D), D_FF ** -0.5),
        'b_dn': nrm(ks[21], (L, N_EXPERTS, D), 0.02),
        'g_final': gain(ks[22], (D,)),
    }


def reference(x_prompt, x_sample, c_prompt, c_sample, w_ada, b_ada, g_mix, g_ffn, w_in, g_q, w_uq,
              g_kv, w_ukv, w_a, w_b, w_out, w_router, b_router, w_gu, b_gu, w_dn, b_dn, g_final):
    y_prompt = trunk(x_prompt, c_prompt, w_ada, b_ada, g_mix, g_ffn, w_in, g_q, w_uq, g_kv, w_ukv,
                     w_a, w_b, w_out, w_router, b_router, w_gu, b_gu, w_dn, b_dn, g_final)
    y_sample = trunk(x_sample, c_sample, w_ada, b_ada, g_mix, g_ffn, w_in, g_q, w_uq, g_kv, w_ukv,
                     w_a, w_b, w_out, w_router, b_router, w_gu, b_gu, w_dn, b_dn, g_final)
    return (y_prompt, y_sample)
```

```python
import numpy as np
import ml_dtypes
from contextlib import ExitStack
import concourse.bass as bass
import concourse.mybir as mybir
from concourse.bass_utils import run_bass_kernel_spmd

F32 = mybir.dt.float32
BF16 = mybir.dt.bfloat16
I32 = mybir.dt.int32
AF = mybir.ActivationFunctionType
ALU = mybir.AluOpType
AX = mybir.AxisListType

T = 8192
D = 1024
NT = 64
NG = 16
H = 8
O1, O2, O3, O4, O5, INW = 384, 640, 672, 1184, 2208, 3232
NE = 32
BLK = 512
NBLK = 96
NSLOT = NBLK * BLK
EPS = 1e-6
ENGS = ("pe", "act", "dve", "pool", "sp")


class Buf:
    __slots__ = ("t", "w", "r", "sem", "excl")

    def __init__(self, t, excl=False):
        self.t = t
        self.excl = excl
        self.w = {}
        self.r = {}
        self.sem = None

    def __getitem__(self, k):
        return self.t[k]


class Ring:
    def __init__(self, bufs):
        self.b = list(bufs)
        self.i = -1

    def next(self):
        self.i = (self.i + 1) % len(self.b)
        return self.b[self.i]


class Prog:
    def __init__(self, nc, st):
        self.nc = nc
        self.st = st
        self.h = {"pe": nc.tensor, "act": nc.scalar, "dve": nc.vector, "pool": nc.gpsimd, "sp": nc.sync}
        self.sems = {e: st.enter_context(nc.semaphore("s_" + e)) for e in ENGS}
        self.cnt = {e: 0 for e in ENGS}
        self.waited = {e: {} for e in ENGS}
        self.dcnt = {}
        self.n_inst = 0
        self.free_sems = []
        self.sem_bufs = []

    def new_sem(self):
        if self.free_sems:
            return self.free_sems.pop()
        k = ("d", len(self.dcnt))
        self.sems[k] = self.st.enter_context(self.nc.semaphore("d_%d" % k[1]))
        self.dcnt[k] = 0
        return k

    def _wait(self, eng, key, val):
        if self.waited[eng].get(key, 0) >= val:
            return
        self.waited[eng][key] = val
        self.h[eng].wait_ge(self.sems[key], val)
        self.n_inst += 1

    def _deps(self, eng, reads, writes, acc, deps):
        need = {}
        for k, v in deps:
            need[k] = max(need.get(k, 0), v)
        for b in reads:
            for k, v in b.w.items():
                need[k] = max(need.get(k, 0), v)
            if b.excl:
                for k, v in b.r.items():
                    if k != eng:
                        need[k] = max(need.get(k, 0), v)
        for b in writes:
            for k, v in b.r.items():
                need[k] = max(need.get(k, 0), v)
            if not acc:
                for k, v in b.w.items():
                    need[k] = max(need.get(k, 0), v)
        for k, v in need.items():
            self._wait(eng, k, v)

    def _record(self, ev, reads, writes, acc):
        k, v = ev
        for b in reads:
            b.r[k] = v
        for b in writes:
            if acc:
                b.w[k] = v
            else:
                b.w = {k: v}
                b.r = {}

    def op(self, eng, fn, reads=(), writes=(), acc=False, deps=()):
        self._deps(eng, reads, writes, acc, deps)
        ins = fn(self.h[eng])
        self.n_inst += 1
        self.cnt[eng] += 1
        ins.then_inc(self.sems[eng], 1)
        ev = (eng, self.cnt[eng])
        self._record(ev, reads, writes, acc)
        return ev

    def mm(self, fns, reads=(), writes=(), acc=False, deps=()):
        self._deps("pe", reads, writes, acc, deps)
        pe = self.h["pe"]
        ins = None
        for fn in fns:
            ins = fn(pe)
            self.n_inst += 1
        self.cnt["pe"] += 1
        ins.then_inc(self.sems["pe"], 1)
        ev = ("pe", self.cnt["pe"])
        self._record(ev, reads, writes, acc)
        return ev

    def dma(self, eng, fn, reads=(), writes=(), acc=False, deps=(), sembuf=None):
        self._deps(eng, reads, writes, acc, deps)
        sb = sembuf if sembuf is not None else (writes[0] if writes else reads[0])
        if sb.sem is None:
            sb.sem = self.new_sem()
            self.sem_bufs.append(sb)
        k = sb.sem
        self.dcnt[k] += 16
        fn(self.h[eng]).then_inc(self.sems[k], 16)
        self.n_inst += 1
        ev = (k, self.dcnt[k])
        self._record(ev, reads, writes, acc)
        return ev

    def barrier(self):
        for k, v in self.dcnt.items():
            self._wait("sp", k, v)
        for e in ENGS:
            if e != "sp":
                self._wait("sp", e, self.cnt[e])
        self.cnt["sp"] += 1
        self.h["sp"].nop().then_inc(self.sems["sp"], 1)
        for e in ENGS:
            if e != "sp":
                self._wait(e, "sp", self.cnt["sp"])
        for e in ENGS:
            for k, v in self.dcnt.items():
                self.waited[e][k] = v
            for e2 in ENGS:
                self.waited[e][e2] = self.cnt[e2]
        for b in self.sem_bufs:
            self.free_sems.append(b.sem)
            b.sem = None
        self.sem_bufs = []


def build_program(depth, stop_after=None, dbg=()):
    nc = bass.Bass("TRN2", target_bir_lowering=False)

    def din(name, shape, dt):
        return nc.dram_tensor(name, list(shape), dt, kind="ExternalInput").ap()

    def dscr(name, shape, dt):
        kind = "ExternalOutput" if name in dbg else "Internal"
        return nc.dram_tensor(name, list(shape), dt, kind=kind).ap()

    L = depth
    x_in = din("x", [T, D], F32)
    c_in = din("c2", [2, D], F32)
    w_ada = din("w_ada", [L, D, 6 * D], F32)
    b_ada = din("b_ada", [L, 6 * D], F32)
    g_mix = din("g_mix", [L, D], F32)
    g_ffn = din("g_ffn", [L, D], F32)
    w_in = din("w_in", [L, D, INW], F32)
    g_q = din("g_q", [L, 384], F32)
    w_uq = din("w_uq", [L, 384, 768], F32)
    g_kv = din("g_kv", [L, 256], F32)
    w_ukv = din("w_ukv", [L, 256, 1024], F32)
    w_a = din("w_a", [L, 512, D], F32)
    w_b = din("w_b", [L, 512, D], F32)
    w_out = din("w_out", [L, D, D], F32)
    w_router = din("w_router", [L, D, NE], F32)
    b_router = din("b_router", [L, NE], F32)
    w_gu = din("w_gu", [L * NE * D, 2 * D], F32)
    b_gu = din("b_gu", [L * NE, 2 * D], F32)
    w_dn = din("w_dn", [L * NE * D, D], F32)
    b_dn = din("b_dn", [L * NE, D], F32)
    g_final = din("g_final", [1, D], F32)
    rope = din("rope", [4, 32, T], F32)
    maskb = din("maskb", [128, 4], F32)
    fcs = din("fcs", [2, NG, 8, 128, 8 * 512], BF16)
    ident_bf_d = din("ident_bf", [128, 128], BF16)
    ident_f_d = din("ident_f", [128, 128], F32)
    ltri_d = din("ltri", [128, 128], BF16)
    bd_d = din("bd", [2, 128, 128], BF16)
    y_out = nc.dram_tensor("y", [T, D], F32, kind="ExternalOutput").ap()

    xmid = dscr("xmid", [T, D], F32)
    xres = dscr("xres", [T, D], F32)
    modd = dscr("modd", [2, 6 * D], F32)
    qT_d = dscr("qT", [H, 96, T], BF16)
    knT_d = dscr("knT", [H, 64, T], BF16)
    krT_d = dscr("krT", [32, T], BF16)
    v_d = dscr("v", [T, 512], BF16)
    uf_d = dscr("uf", [T, 512], BF16)
    gT_d = dscr("gT", [16, 128, T], BF16)
    aoT_d = dscr("aoT", [512, T], BF16)
    yfT_d = dscr("yfT", [2, 512, T], BF16)
    h2_d = dscr("h2", [T, D], BF16)
    hs_d = dscr("hs", [NSLOT, D], BF16)
    ys_d = dscr("ys", [NSLOT, D], BF16)

    with ExitStack() as st:
        P = Prog(nc, st)

        uid = [0]

        def sbuf(ctx, name, shape, dt):
            uid[0] += 1
            return Buf(ctx.enter_context(nc.sbuf_tensor("sb%d_%s" % (uid[0], name), list(shape), dt)))

        def psum(ctx, name, shape, dt):
            uid[0] += 1
            return Buf(ctx.enter_context(nc.psum_tensor("ps%d_%s" % (uid[0], name), list(shape), dt)), excl=True)

        ident_bf = sbuf(st, "ident_bf", [128, 128], BF16)
        ident_f = sbuf(st, "ident_f", [128, 128], F32)
        ltri = sbuf(st, "ltri", [128, 128], BF16)
        ones_bf = sbuf(st, "ones_bf", [128, 512], BF16)
        ones_f = sbuf(st, "ones_f", [128, 128], F32)
        eps_t = sbuf(st, "eps_t", [128, 1], F32)
        mb = sbuf(st, "mb", [128, 4], F32)
        scT = sbuf(st, "scT", [128, 8, 2], F32)
        P.dma("sp", lambda e: e.dma_start(out=ident_bf[:], in_=ident_bf_d[:, :]), writes=[ident_bf])
        P.dma("sp", lambda e: e.dma_start(out=ident_f[:], in_=ident_f_d[:, :]), writes=[ident_f])
        P.dma("sp", lambda e: e.dma_start(out=ltri[:], in_=ltri_d[:, :]), writes=[ltri])
        P.dma("sp", lambda e: e.dma_start(out=mb[:], in_=maskb[:, :]), writes=[mb])
        P.op("dve", lambda e: e.memset(ones_bf[:], 1.0), writes=[ones_bf])
        P.op("dve", lambda e: e.memset(ones_f[:], 1.0), writes=[ones_f])
        P.op("dve", lambda e: e.memset(eps_t[:], EPS), writes=[eps_t])
        for s in range(2):
            P.dma("sp", lambda e, s=s: e.dma_start(
                out=scT[:, :, s], in_=c_in[s, :].rearrange("(j p) -> p j", p=128),
                allow_slow_non_contiguous=True), writes=[scT], acc=True)
        P.op("act", lambda e: e.activation(out=scT[:], in_=scT[:], func=AF.Silu), reads=[scT], writes=[scT])
        modT = sbuf(st, "modT", [128, 48, 2], F32)
        gs1T = sbuf(st, "gs1T", [128, 8, 2], F32)
        gmT = sbuf(st, "gmT", [128, 8], F32)
        gqT = sbuf(st, "gqT", [128, 3], F32)
        gkvT = sbuf(st, "gkvT", [128, 2], F32)
        P.barrier()

        def cast_load(dst, dst_ap_fn, src_rows, ncols, nchunk, col0=0, writes_acc=False):
            first = True
            for c in range(nchunk):
                for cc in range(0, ncols, 2048):
                    w = min(2048, ncols - cc)
                    P.dma("pool", lambda e, c=c, cc=cc, w=w: e.dma_start(
                        out=dst_ap_fn(c, cc, w), in_=src_rows[c * 128:(c + 1) * 128, col0 + cc:col0 + cc + w]),
                        writes=[dst], acc=(not first) or writes_acc)
                    first = False

        for l in range(L):
            x_src = x_in if l == 0 else xres
            last = (l == L - 1)
            with ExitStack() as ph:
                ba2 = sbuf(ph, "ba2", [2, 6 * D], F32)
                mod_sb = sbuf(ph, "mod_sb", [2, 6 * D], F32)
                wa_r = Ring([sbuf(ph, "wa%d" % i, [128, 8, 512], F32) for i in range(2)])
                pm_r = Ring([psum(ph, "pm%d" % i, [2, 512], F32) for i in range(2)])
                P.dma("sp", lambda e: e.dma_start(out=ba2[:], in_=b_ada[l:l + 1, :].partition_broadcast(2)), writes=[ba2])
                for ng in range(12):
                    wa = wa_r.next()
                    pm = pm_r.next()
                    P.dma("sp", lambda e: e.dma_start(
                        out=wa[:], in_=w_ada[l, :, ng * 512:(ng + 1) * 512].rearrange("(j p) n -> p j n", p=128)),
                        writes=[wa])
                    P.mm([lambda e, j=j: e.matmul(pm[:], lhsT=scT[:, j, :], rhs=wa[:, j, :], start=(j == 0), stop=(j == 7))
                          for j in range(8)], reads=[scT, wa], writes=[pm])
                    P.op("dve", lambda e: e.tensor_tensor(out=mod_sb[:, ng * 512:(ng + 1) * 512], in0=pm[:],
                                                         in1=ba2[:, ng * 512:(ng + 1) * 512], op=ALU.add),
                         reads=[pm, ba2], writes=[mod_sb], acc=(ng > 0))
                P.dma("sp", lambda e: e.dma_start(out=modd[:, :], in_=mod_sb[:]), reads=[mod_sb])
                P.barrier()
                for s in range(2):
                    P.dma("sp", lambda e, s=s: e.dma_start(
                        out=modT[:, :, s], in_=modd[s, :].rearrange("(j p) -> p j", p=128),
                        allow_slow_non_contiguous=True), writes=[modT], acc=(s > 0))
                P.dma("sp", lambda e: e.dma_start(out=gmT[:], in_=g_mix[l, :].rearrange("(j p) -> p j", p=128),
                                                  allow_slow_non_contiguous=True), writes=[gmT])
                P.dma("sp", lambda e: e.dma_start(out=gqT[:], in_=g_q[l, :].rearrange("(j p) -> p j", p=128),
                                                  allow_slow_non_contiguous=True), writes=[gqT])
                P.dma("sp", lambda e: e.dma_start(out=gkvT[:], in_=g_kv[l, :].rearrange("(j p) -> p j", p=128),
                                                  allow_slow_non_contiguous=True), writes=[gkvT])
                for s in range(2):
                    P.op("dve", lambda e, s=s: e.scalar_tensor_tensor(out=gs1T[:, :, s], in0=modT[:, 8:16, s], scalar=1.0,
                                                                     in1=gmT[:], op0=ALU.add, op1=ALU.mult),
                         reads=[modT, gmT], writes=[gs1T], acc=(s > 0))
                P.barrier()
            if stop_after == "p0":
                break

            with ExitStack() as ph:
                win = sbuf(ph, "win", [128, 8, INW], BF16)
                wuq = sbuf(ph, "wuq", [128, 3, 768], BF16)
                wuqr = sbuf(ph, "wuqr", [128, 3, 768], BF16)
                wukv = sbuf(ph, "wukv", [128, 2, 1024], BF16)
                wkr = sbuf(ph, "wkr", [128, 8, 96], BF16)
                wkrr = sbuf(ph, "wkrr", [128, 8, 96], BF16)
                cast_load(win, lambda c, cc, w: win[:, c, cc:cc + w], w_in[l], INW, 8)
                cast_load(wuq, lambda c, cc, w: wuq[:, c, cc:cc + w], w_uq[l], 768, 3)
                cast_load(wukv, lambda c, cc, w: wukv[:, c, cc:cc + w], w_ukv[l], 1024, 2)
                P.op("pool", lambda e: e.memset(wuqr[:], 0.0), writes=[wuqr])
                P.op("pool", lambda e: e.memset(wkr[:], 0.0), writes=[wkr])
                P.op("pool", lambda e: e.memset(wkrr[:], 0.0), writes=[wkrr])
                wuq4 = lambda t: t[:].rearrange("p c (h r) -> p c h r", h=8)
                P.op("dve", lambda e: e.tensor_scalar(out=wuq4(wuqr)[:, :, :, 64:80], in0=wuq4(wuq)[:, :, :, 80:96],
                                                     scalar1=-1.0, scalar2=None, op0=ALU.mult),
                     reads=[wuq], writes=[wuqr], acc=True, deps=list(wuqr.w.items()))
                P.op("dve", lambda e: e.tensor_copy(out=wuq4(wuqr)[:, :, :, 80:96], in_=wuq4(wuq)[:, :, :, 64:80]),
                     reads=[wuq], writes=[wuqr], acc=True)
                P.op("dve", lambda e: e.tensor_copy(out=wkr[:, :, 64:96], in_=win[:, :, O2:O3]),
                     reads=[win], writes=[wkr], acc=True, deps=list(wkr.w.items()))
                P.op("dve", lambda e: e.tensor_scalar(out=wkrr[:, :, 64:80], in0=win[:, :, O2 + 16:O3],
                                                     scalar1=-1.0, scalar2=None, op0=ALU.mult),
                     reads=[win], writes=[wkrr], acc=True, deps=list(wkrr.w.items()))
                P.op("dve", lambda e: e.tensor_copy(out=wkrr[:, :, 80:96], in_=win[:, :, O2:O2 + 16]),
                     reads=[win], writes=[wkrr], acc=True)

                x_r = Ring([sbuf(ph, "xt%d" % i, [128, D], F32) for i in range(2)])
                sq_scr = sbuf(ph, "sq_scr", [128, D], BF16)
                ss_r = Ring([sbuf(ph, "ss%d" % i, [128, 2], F32) for i in range(3)])
                xn_r = Ring([sbuf(ph, "xn%d" % i, [128, D], BF16) for i in range(4)])
                pT_r = Ring([psum(ph, "pT%d" % i, [128, 4, 512], BF16) for i in range(1)])
                hT_r = Ring([sbuf(ph, "hT%d" % i, [128, 8, 512], BF16) for i in range(2)])
                pu_r = Ring([psum(ph, "pu%d" % i, [128, 512], F32) for i in range(4)])
                pq_r = Ring([psum(ph, "pq%d" % i, [128, 512], F32) for i in range(2)])
                uq_sb = sbuf(ph, "uq_sb", [128, 5, 512], F32)
                sq_sb = sbuf(ph, "sq_sb", [128, 5, 512], BF16)
                rs_r = Ring([sbuf(ph, "rs%d" % i, [128, 512], F32) for i in range(2)])
                qn = sbuf(ph, "qn", [128, 5, 512], BF16)
                qT_sb_r = Ring([sbuf(ph, "qT_sb%d" % i, [96, 8, 512], BF16) for i in range(1)])
                knT_sb_r = Ring([sbuf(ph, "knT_sb%d" % i, [64, 8, 512], BF16) for i in range(1)])
                krT_sb_r = Ring([sbuf(ph, "krT_sb%d" % i, [96, 512], BF16) for i in range(2)])
                rt_r = Ring([sbuf(ph, "rt%d" % i, [96, 2, 512], F32) for i in range(2)])
                v_sb_r = Ring([sbuf(ph, "v_sb%d" % i, [128, 512], BF16) for i in range(2)])
                f_sb_r = Ring([sbuf(ph, "f_sb%d" % i, [128, 512], BF16) for i in range(2)])
                g_sb_r = Ring([sbuf(ph, "g_sb%d" % i, [128, 512], BF16) for i in range(3)])
                tab_r = Ring([sbuf(ph, "tab%d" % i, [96, 4, 512], F32) for i in range(1)])

                for g in range(NG):
                    slot = g // 8
                    cols = slice(g * 512, (g + 1) * 512)
                    tab = tab_r.next()
                    P.dma("sp", lambda e: e.dma_start(out=tab[64:96, :, :], in_=rope[:, :, cols].rearrange("t p n -> p t n")),
                          writes=[tab])
                    hT = hT_r.next()
                    for half in range(2):
                        pT = pT_r.next()
                        if half == 0:
                            xns = []
                            for ti in range(4):
                                t = g * 4 + ti
                                xt = x_r.next(); ss = ss_r.next(); xn = xn_r.next()
                                P.dma("sp", lambda e: e.dma_start(out=xt[:], in_=x_src[t * 128:(t + 1) * 128, :]), writes=[xt])
                                P.op("act", lambda e: e.activation(out=sq_scr[:], in_=xt[:], func=AF.Square, accum_out=ss[:, 0:1]),
                                     reads=[xt], writes=[sq_scr, ss])
                                P.op("act", lambda e: e.activation(out=ss[:, 1:2], in_=ss[:, 0:1], func=AF.Sqrt, scale=1.0 / D, bias=eps_t[:, 0:1]),
                                     reads=[ss, eps_t], writes=[ss], acc=True)
                                P.op("dve", lambda e: e.reciprocal(out=ss[:, 1:2], in_=ss[:, 1:2]), reads=[ss], writes=[ss], acc=True)
                                P.op("dve", lambda e: e.tensor_scalar(out=xn[:], in0=xt[:], scalar1=ss[:, 1:2], scalar2=None, op0=ALU.mult),
                                     reads=[xt, ss], writes=[xn])
                                xns.append(xn)
                        first = True
                        for cj in range(4):
                            c = half * 4 + cj
                            for ti in range(4):
                                P.mm([lambda e: e.transpose(out=pT[:, cj, ti * 128:(ti + 1) * 128],
                                                            in_=xns[ti][:, c * 128:(c + 1) * 128], identity=ident_bf[:])],
                                     reads=[xns[ti], ident_bf], writes=[pT], acc=not first)
                                first = False
                        for cj in range(4):
                            c = half * 4 + cj
                            P.op("act", lambda e: e.activation(out=hT[:, c, :], in_=pT[:, cj, :], func=AF.Identity,
                                                               scale=gs1T[:, c, slot:slot + 1], bias=modT[:, c, slot:slot + 1]),
                                 reads=[pT, gs1T, modT], writes=[hT], acc=not (half == 0 and cj == 0))

                    if stop_after == "pA.hT":
                        break
                    def inproj(col0, m, lhs=None):
                        pu = pu_r.next()
                        wsrc = win if lhs is None else lhs
                        P.mm([lambda e, c=c: e.matmul(pu[0:m, :], lhsT=wsrc[:, c, col0:col0 + m], rhs=hT[:, c, :],
                                                      start=(c == 0), stop=(c == 7)) for c in range(8)],
                             reads=[wsrc, hT], writes=[pu])
                        return pu

                    for j in range(5):
                        pu = inproj(j * 128, 128)
                        import os
                        if "D" not in os.environ.get("DBGSKIP", ""):
                            P.op("dve", lambda e: e.tensor_copy(out=uq_sb[:, j, :], in_=pu[:]), reads=[pu], writes=[uq_sb], acc=(j > 0))
                        if "A" not in os.environ.get("DBGSKIP", ""):
                            P.op("act", lambda e: e.activation(out=sq_sb[:, j, :], in_=pu[:], func=AF.Square), reads=[pu], writes=[sq_sb], acc=(j > 0))
                    if stop_after == "pA.lat1":
                        break
                    for (j0, nj, width, gT_) in ((0, 3, 384, gqT), (3, 2, 256, gkvT)):
                        pq = pq_r.next(); rs = rs_r.next()
                        P.mm([lambda e, j=j: e.matmul(pq[:], lhsT=ones_bf[:, 0:128], rhs=sq_sb[:, j0 + j, :],
                                                      start=(j == 0), stop=(j == nj - 1)) for j in range(nj)],
                             reads=[ones_bf, sq_sb], writes=[pq])
                        P.op("act", lambda e: e.activation(out=rs[:], in_=pq[:], func=AF.Sqrt, scale=1.0 / width, bias=eps_t[:, 0:1]),
                             reads=[pq, eps_t], writes=[rs])
                        P.op("dve", lambda e: e.reciprocal(out=rs[:], in_=rs[:]), reads=[rs], writes=[rs])
                        if stop_after == "pA.lat2":
                            break
                        for j in range(nj):
                            P.op("dve", lambda e, j=j: e.scalar_tensor_tensor(out=qn[:, j0 + j, :], in0=uq_sb[:, j0 + j, :],
                                                                             scalar=gT_[:, j:j + 1], in1=rs[:], op0=ALU.mult, op1=ALU.mult),
                                 reads=[uq_sb, rs, gT_], writes=[qn], acc=not (j0 == 0 and j == 0))
                    if stop_after == "pA.lat":
                        break
                    qT_sb = qT_sb_r.next()
                    for h in range(H):
                        pq = pq_r.next(); pqr = pq_r.next(); rt = rt_r.next()
                        P.mm([lambda e, c=c: e.matmul(pq[0:96, :], lhsT=wuq[:, c, h * 96:(h + 1) * 96], rhs=qn[:, c, :],
                                                      start=(c == 0), stop=(c == 2)) for c in range(3)],
                             reads=[wuq, qn], writes=[pq])
                        P.mm([lambda e, c=c: e.matmul(pqr[0:96, :], lhsT=wuqr[:, c, h * 96:(h + 1) * 96], rhs=qn[:, c, :],
                                                      start=(c == 0), stop=(c == 2)) for c in range(3)],
                             reads=[wuqr, qn], writes=[pqr])
                        P.op("act", lambda e: e.activation(out=qT_sb[0:64, h, :], in_=pq[0:64, :], func=AF.Copy, scale=96.0 ** -0.5),
                             reads=[pq], writes=[qT_sb], acc=(h > 0))
                        P.op("dve", lambda e: e.tensor_tensor(out=rt[64:96, 0, :], in0=pq[64:96, :], in1=tab[64:96, 0, :], op=ALU.mult),
                             reads=[pq, tab], writes=[rt])
                        P.op("dve", lambda e: e.tensor_tensor(out=rt[64:96, 1, :], in0=pqr[64:96, :], in1=tab[64:96, 1, :], op=ALU.mult),
                             reads=[pqr, tab], writes=[rt], acc=True)
                        P.op("dve", lambda e: e.tensor_tensor(out=qT_sb[64:96, h, :], in0=rt[64:96, 0, :], in1=rt[64:96, 1, :], op=ALU.add),
                             reads=[rt], writes=[qT_sb], acc=True)
                    P.dma("sp", lambda e: e.dma_start(out=qT_d[:, :, cols].rearrange("h r n -> r h n"), in_=qT_sb[:]), reads=[qT_sb])
                    if stop_after == "pA.q":
                        break
                    knT_sb = knT_sb_r.next()
                    for h in range(H):
                        pq = pq_r.next()
                        P.mm([lambda e, c=c: e.matmul(pq[0:64, :], lhsT=wukv[:, c, h * 128:h * 128 + 64], rhs=qn[:, 3 + c, :],
                                                      start=(c == 0), stop=(c == 1)) for c in range(2)],
                             reads=[wukv, qn], writes=[pq])
                        P.op("act", lambda e: e.activation(out=knT_sb[:, h, :], in_=pq[0:64, :], func=AF.Copy),
                             reads=[pq], writes=[knT_sb], acc=(h > 0))
                    P.dma("sp", lambda e: e.dma_start(out=knT_d[:, :, cols].rearrange("h r n -> r h n"), in_=knT_sb[:]), reads=[knT_sb])
                    if stop_after == "pA.kn":
                        break
                    krT_sb = krT_sb_r.next(); rt = rt_r.next()
                    pk = inproj(0, 96, lhs=wkr)
                    pkr = inproj(0, 96, lhs=wkrr)
                    P.op("dve", lambda e: e.tensor_tensor(out=rt[64:96, 0, :], in0=pk[64:96, :], in1=tab[64:96, 2, :], op=ALU.mult),
                         reads=[pk, tab], writes=[rt])
                    P.op("dve", lambda e: e.tensor_tensor(out=rt[64:96, 1, :], in0=pkr[64:96, :], in1=tab[64:96, 3, :], op=ALU.mult),
                         reads=[pkr, tab], writes=[rt], acc=True)
                    P.op("dve", lambda e: e.tensor_tensor(out=krT_sb[64:96, :], in0=rt[64:96, 0, :], in1=rt[64:96, 1, :], op=ALU.add),
                         reads=[rt], writes=[krT_sb])
                    P.dma("sp", lambda e: e.dma_start(out=krT_d[:, cols], in_=krT_sb[64:96, :]), reads=[krT_sb])
                    if stop_after == "pA.kr":
                        break
                    wv = wukv[:].rearrange("p c (h r) -> p c h r", h=8)
                    for ti in range(4):
                        t = g * 4 + ti
                        pu = pu_r.next(); v_sb = v_sb_r.next()
                        P.mm([lambda e, c=c: e.matmul(pu[:].rearrange("p (h r) -> p h r", h=8), lhsT=qn[:, 3 + c, ti * 128:(ti + 1) * 128],
                                                      rhs=wv[:, c, :, 64:128], start=(c == 0), stop=(c == 1)) for c in range(2)],
                             reads=[wukv, qn], writes=[pu])
                        P.op("act", lambda e: e.activation(out=v_sb[:], in_=pu[:], func=AF.Copy), reads=[pu], writes=[v_sb])
                        P.dma("sp", lambda e: e.dma_start(out=v_d[t * 128:(t + 1) * 128, :], in_=v_sb[:]), reads=[v_sb])
                        pu = pu_r.next(); f_sb = f_sb_r.next()
                        P.mm([lambda e, c=c: e.matmul(pu[:], lhsT=hT[:, c, ti * 128:(ti + 1) * 128], rhs=win[:, c, O3:O4],
                                                      start=(c == 0), stop=(c == 7)) for c in range(8)],
                             reads=[win, hT], writes=[pu])
                        P.op("dve", lambda e: e.tensor_copy(out=f_sb[:], in_=pu[:]), reads=[pu], writes=[f_sb])
                        P.dma("sp", lambda e: e.dma_start(out=uf_d[t * 128:(t + 1) * 128, :], in_=f_sb[:]), reads=[f_sb])
                    if stop_after == "pA.v":
                        break
                    for j in range(16):
                        pu = inproj(O4 + j * 128, 128)
                        g_sb = g_sb_r.next()
                        P.op("act", lambda e: e.activation(out=g_sb[:], in_=pu[:], func=AF.Sigmoid), reads=[pu], writes=[g_sb])
                        P.dma("sp", lambda e: e.dma_start(out=gT_d[j, :, cols], in_=g_sb[:]), reads=[g_sb])
                P.barrier()
            if stop_after == "pA":
                break

            with ExitStack() as ph:
                kT_r = Ring([sbuf(ph, "kT%d" % i, [96, T], BF16) for i in range(2)])
                qT_r = Ring([sbuf(ph, "qTb%d" % i, [96, T], BF16) for i in range(2)])
                vp_r = Ring([sbuf(ph, "vp%d" % i, [128, NT, 128], BF16) for i in range(2)])
                ps_r = Ring([psum(ph, "psS%d" % i, [128, 512], F32) for i in range(3)])
                po_r = Ring([psum(ph, "psO%d" % i, [128, 512], F32) for i in range(2)])
                pt_r = Ring([sbuf(ph, "pt%d" % i, [128, 512], BF16) for i in range(3)])
                r_r = Ring([sbuf(ph, "rr%d" % i, [64, 512], F32) for i in range(2)])
                o_r = Ring([sbuf(ph, "oo%d" % i, [64, 512], BF16) for i in range(2)])
                for vp in vp_r.b:
                    P.op("pool", lambda e: e.memset(vp[:], 1.0), writes=[vp])

                def load_head(h):
                    kT = kT_r.next(); qT = qT_r.next(); vp = vp_r.next()
                    P.dma("sp", lambda e: e.dma_start(out=kT[0:64, :], in_=knT_d[h, :, :]), writes=[kT])
                    P.dma("sp", lambda e: e.dma_start(out=kT[64:96, :], in_=krT_d[:, :]), writes=[kT], acc=True)
                    P.dma("sp", lambda e: e.dma_start(out=qT[:], in_=qT_d[h, :, :]), writes=[qT])
                    P.dma("sp", lambda e: e.dma_start(out=vp[:, :, 0:64],
                                                      in_=v_d[:, h * 64:(h + 1) * 64].rearrange("(t p) c -> p t c", p=128)),
                          writes=[vp], acc=True, deps=list(vp.r.items()) + list(vp.w.items()))
                    return kT, qT, vp

                steps = [(h, qc, kt) for h in range(H) for qc in range(NG) for kt in range(NT)]
                heads = {0: load_head(0)}
                st_ps = {}

                def emit_qk(i):
                    h, qc, kt = steps[i]
                    if h not in heads:
                        heads[h] = load_head(h)
                    kT, qT, vp = heads[h]
                    ps = ps_r.next()
                    P.mm([lambda e: e.matmul(ps[:], lhsT=kT[:, kt * 128:(kt + 1) * 128], rhs=qT[:, qc * 512:(qc + 1) * 512],
                                             start=True, stop=True)], reads=[kT, qT], writes=[ps])
                    st_ps[i] = ps

                LOOK = 2
                for i in range(min(LOOK, len(steps))):
                    emit_qk(i)
                po = None
                for i, (h, qc, kt) in enumerate(steps):
                    if i + LOOK < len(steps):
                        emit_qk(i + LOOK)
                    kT, qT, vp = heads[h]
                    if kt == 0:
                        po = po_r.next()
                        if qc == 0 and h + 1 < H and (h + 1) not in heads:
                            heads[h + 1] = load_head(h + 1)
                    ps = st_ps.pop(i)
                    pt = pt_r.next()
                    mi = 2 * (kt // 32) + (qc // 8)
                    P.op("act", lambda e: e.activation(out=pt[:], in_=ps[:], func=AF.Exp, bias=mb[:, mi:mi + 1]),
                         reads=[ps, mb], writes=[pt])
                    P.mm([lambda e: e.matmul(po[:], lhsT=vp[:, kt, :], rhs=pt[:], start=(kt == 0), stop=(kt == NT - 1))],
                         reads=[vp, pt], writes=[po], acc=(kt > 0))
                    if kt == NT - 1:
                        rr = r_r.next(); oo = o_r.next()
                        P.op("dve", lambda e: e.reciprocal(out=rr[:], in_=po[64:128, :]), reads=[po], writes=[rr])
                        P.op("dve", lambda e: e.tensor_tensor(out=oo[:], in0=po[0:64, :], in1=rr[:], op=ALU.mult),
                             reads=[po, rr], writes=[oo])
                        P.dma("sp", lambda e: e.dma_start(out=aoT_d[h * 64:(h + 1) * 64, qc * 512:(qc + 1) * 512], in_=oo[:]), reads=[oo])
                P.barrier()
            if stop_after == "pB":
                break

            with ExitStack() as ph:
                z = sbuf(ph, "z", [128, NT, 512], BF16)
                f_r = Ring([sbuf(ph, "fb%d" % i, [128, 2, 8 * 512], BF16) for i in range(3)])
                pacc = [psum(ph, "pacc%d" % i, [128, 512], F32) for i in range(8)]
                y_r = Ring([sbuf(ph, "ysb%d" % i, [128, 512], BF16) for i in range(4)])
                P.dma("sp", lambda e: e.dma_start(out=z[:], in_=uf_d[:, :].rearrange("(t p) c -> p t c", p=128)), writes=[z])
                for kc in range(NG):
                    for slab in range(8):
                        fb = f_r.next()
                        P.dma("sp", lambda e: e.dma_start(out=fb[:, 0, :], in_=fcs[0, kc, slab, :, :]), writes=[fb])
                        P.dma("sp", lambda e: e.dma_start(out=fb[:, 1, :], in_=fcs[1, kc, slab, :, :]), writes=[fb], acc=True)
                        fns = []
                        for j in range(8):
                            nt = slab * 8 + j
                            for cc in range(4):
                                for ri in range(2):
                                    fns.append(lambda e, nt=nt, j=j, cc=cc, ri=ri: e.matmul(
                                        pacc[ri * 4 + cc][:], lhsT=z[:, nt, cc * 128:(cc + 1) * 128],
                                        rhs=fb[:, ri, j * 512:(j + 1) * 512], start=(nt == 0), stop=(nt == NT - 1)))
                        P.mm(fns, reads=[z, fb], writes=pacc, acc=(slab > 0))
                    for i in range(8):
                        ri, cc = i // 4, i % 4
                        ysb = y_r.next()
                        if i % 2 == 0:
                            P.op("act", lambda e: e.activation(out=ysb[:], in_=pacc[i][:], func=AF.Copy), reads=[pacc[i]], writes=[ysb])
                        else:
                            P.op("dve", lambda e: e.tensor_copy(out=ysb[:], in_=pacc[i][:]), reads=[pacc[i]], writes=[ysb])
                        P.dma("sp", lambda e: e.dma_start(out=yfT_d[ri, cc * 128:(cc + 1) * 128, kc * 512:(kc + 1) * 512], in_=ysb[:]), reads=[ysb])
                P.barrier()
            if stop_after == "pC":
                break

            lay = ExitStack()
            lg_all = sbuf(lay, "lg_all", [128, NT, NE], F32)
            with ExitStack() as ph:
                wa = sbuf(ph, "wa_", [128, 4, D], BF16)
                wb = sbuf(ph, "wb_", [128, 4, D], BF16)
                wbc = sbuf(ph, "wbc", [128, 4, D], BF16)
                wbs = sbuf(ph, "wbs", [128, 4, D], BF16)
                wout = sbuf(ph, "wout", [128, 8, D], BF16)
                wr = sbuf(ph, "wr", [128, 8, NE], F32)
                brt = sbuf(ph, "brt", [128, NE], F32)
                bd = sbuf(ph, "bd", [128, 2, 128], BF16)
                ga1_b = [sbuf(ph, "ga1_b%d" % i, [128, D], F32) for i in range(2)]
                sh2_b = [sbuf(ph, "sh2_b%d" % i, [128, D], F32) for i in range(2)]
                gs2_b = [sbuf(ph, "gs2_b%d" % i, [128, D], F32) for i in range(2)]
                gff_b = sbuf(ph, "gff_b", [128, D], F32)
                pab_r = Ring([psum(ph, "pab%d" % i, [128, 512], F32) for i in range(3)])
                po_r = Ring([psum(ph, "pod%d" % i, [128, 512], F32) for i in range(2)])
                pT32 = psum(ph, "pT32", [128, 8, 128], F32)
                plg = psum(ph, "plg", [128, 512], F32)
                cast_load(wa, lambda c, cc, w: wa[:, c, cc:cc + w], w_a[l], D, 4)
                cast_load(wb, lambda c, cc, w: wb[:, c, cc:cc + w], w_b[l], D, 4)
                cast_load(wout, lambda c, cc, w: wout[:, c, cc:cc + w], w_out[l], D, 8)
                P.dma("sp", lambda e: e.dma_start(out=wr[:], in_=w_router[l, :, :].rearrange("(c p) n -> p c n", p=128)), writes=[wr])
                P.dma("sp", lambda e: e.dma_start(out=brt[:], in_=b_router[l:l + 1, :].partition_broadcast(128)), writes=[brt])
                P.dma("sp", lambda e: e.dma_start(out=bd[:], in_=bd_d[:, :, :].rearrange("i p n -> p i n")), writes=[bd])
                P.dma("sp", lambda e: e.dma_start(out=gff_b[:], in_=g_ffn[l:l + 1, :].partition_broadcast(128)), writes=[gff_b])
                for s_ in range(2):
                    P.dma("sp", lambda e: e.dma_start(out=ga1_b[s_][:], in_=modd[s_:s_ + 1, 2 * D:3 * D].partition_broadcast(128)), writes=[ga1_b[s_]])
                    P.dma("sp", lambda e: e.dma_start(out=sh2_b[s_][:], in_=modd[s_:s_ + 1, 3 * D:4 * D].partition_broadcast(128)), writes=[sh2_b[s_]])
                    P.dma("sp", lambda e: e.dma_start(out=gs2_b[s_][:], in_=modd[s_:s_ + 1, 4 * D:5 * D].partition_broadcast(128)), writes=[gs2_b[s_]])
                    P.op("dve", lambda e: e.scalar_tensor_tensor(out=gs2_b[s_][:], in0=gs2_b[s_][:], scalar=1.0, in1=gff_b[:],
                                                               op0=ALU.add, op1=ALU.mult), reads=[gs2_b[s_], gff_b], writes=[gs2_b[s_]])
                for i, dst in enumerate((wbc, wbs)):
                    for c in range(4):
                        for half in range(2):
                            po = po_r.next()
                            P.mm([lambda e: e.matmul(po[:], lhsT=bd[:, i, :], rhs=wb[:, c, half * 512:(half + 1) * 512], start=True, stop=True)],
                                 reads=[bd, wb], writes=[po])
                            P.op("act", lambda e: e.activation(out=dst[:, c, half * 512:(half + 1) * 512], in_=po[:], func=AF.Copy),
                                 reads=[po], writes=[dst], acc=not (c == 0 and half == 0))
                ao_r = Ring([sbuf(ph, "ao%d" % i, [128, 4, 512], BF16) for i in range(2)])
                yr_r = Ring([sbuf(ph, "yr%d" % i, [128, 2, 4, 512], BF16) for i in range(2)])
                gg_r = Ring([sbuf(ph, "gg%d" % i, [128, 16, 512], BF16) for i in range(2)])
                t1_r = Ring([sbuf(ph, "t1_%d" % i, [128, 512], F32) for i in range(2)])
                t2_r = Ring([sbuf(ph, "t2_%d" % i, [128, 512], F32) for i in range(2)])
                mg_r = Ring([sbuf(ph, "mg%d" % i, [128, 8, 512], BF16) for i in range(2)])
                xd_r = Ring([sbuf(ph, "xd%d" % i, [128, D], F32) for i in range(2)])
                x1_r = Ring([sbuf(ph, "x1_%d" % i, [128, D], F32) for i in range(2)])
                h2f_r = Ring([sbuf(ph, "h2f%d" % i, [128, D], F32) for i in range(2)])
                h2b_r = Ring([sbuf(ph, "h2b%d" % i, [128, D], BF16) for i in range(2)])
                h2T_r = Ring([sbuf(ph, "h2T%d" % i, [128, 8, 128], F32) for i in range(2)])
                sqd = sbuf(ph, "sqd", [128, D], BF16)
                ssd_r = Ring([sbuf(ph, "ssd%d" % i, [128, 2], F32) for i in range(2)])

                def load_group(g):
                    cols = slice(g * 512, (g + 1) * 512)
                    ao = ao_r.next(); yr = yr_r.next(); gg = gg_r.next()
                    P.dma("sp", lambda e: e.dma_start(out=ao[:], in_=aoT_d[:, cols].rearrange("(c p) n -> p c n", p=128)), writes=[ao])
                    for ri in range(2):
                        P.dma("sp", lambda e: e.dma_start(out=yr[:, ri, :, :], in_=yfT_d[ri, :, cols].rearrange("(c p) n -> p c n", p=128)),
                              writes=[yr], acc=(ri > 0))
                    P.dma("sp", lambda e: e.dma_start(out=gg[:], in_=gT_d[:, :, cols].rearrange("j p n -> p j n")), writes=[gg])
                    return ao, yr, gg

                nxt = load_group(0)
                for g in range(NG):
                    slot = g // 8
                    ao, yr, gg = nxt
                    if g + 1 < NG:
                        nxt = load_group(g + 1)
                    mg = mg_r.next()
                    for dc in range(8):
                        dcs = slice(dc * 128, (dc + 1) * 128)
                        pa = pab_r.next()
                        P.mm([lambda e, c=c: e.matmul(pa[:], lhsT=wa[:, c, dcs], rhs=ao[:, c, :], start=(c == 0), stop=(c == 3))
                              for c in range(4)], reads=[wa, ao], writes=[pa])
                        pb = pab_r.next()
                        P.mm([lambda e, c=c: e.matmul(pb[:], lhsT=(wbc if c < 4 else wbs)[:, c % 4, dcs], rhs=yr[:, c // 4, c % 4, :],
                                                      start=(c == 0), stop=(c == 7)) for c in range(8)], reads=[wbc, wbs, yr], writes=[pb])
                        t1 = t1_r.next(); t2 = t2_r.next()
                        P.op("dve", lambda e: e.tensor_tensor(out=t1[:], in0=pa[:], in1=gg[:, dc, :], op=ALU.mult), reads=[pa, gg], writes=[t1])
                        P.op("dve", lambda e: e.tensor_tensor(out=t2[:], in0=pb[:], in1=gg[:, 8 + dc, :], op=ALU.mult), reads=[pb, gg], writes=[t2])
                        P.op("pool", lambda e: e.tensor_tensor(out=mg[:, dc, :], in0=t1[:], in1=t2[:], op=ALU.add),
                             reads=[t1, t2], writes=[mg], acc=(dc > 0))
                    for ti in range(4):
                        t = g * 4 + ti
                        rows = slice(t * 128, (t + 1) * 128)
                        xd = xd_r.next(); x1 = x1_r.next()
                        P.dma("sp", lambda e: e.dma_start(out=xd[:], in_=x_src[rows, :]), writes=[xd])
                        for half in range(2):
                            hs_ = slice(half * 512, (half + 1) * 512)
                            po = po_r.next()
                            P.mm([lambda e, dc=dc: e.matmul(po[:], lhsT=mg[:, dc, ti * 128:(ti + 1) * 128], rhs=wout[:, dc, hs_],
                                                            start=(dc == 0), stop=(dc == 7)) for dc in range(8)], reads=[mg, wout], writes=[po])
                            P.op("dve", lambda e: e.tensor_tensor(out=x1[:, hs_], in0=po[:], in1=ga1_b[slot][:, hs_], op=ALU.mult),
                                 reads=[po, ga1_b[slot]], writes=[x1], acc=(half > 0))
                        P.op("pool", lambda e: e.tensor_tensor(out=x1[:], in0=x1[:], in1=xd[:], op=ALU.add), reads=[x1, xd], writes=[x1])
                        P.dma("sp", lambda e: e.dma_start(out=xmid[rows, :], in_=x1[:]), reads=[x1])
                        ss = ssd_r.next(); h2f = h2f_r.next(); h2b = h2b_r.next(); h2T = h2T_r.next()
                        P.op("act", lambda e: e.activation(out=sqd[:], in_=x1[:], func=AF.Square, accum_out=ss[:, 0:1]), reads=[x1], writes=[sqd, ss])
                        P.op("act", lambda e: e.activation(out=ss[:, 1:2], in_=ss[:, 0:1], func=AF.Sqrt, scale=1.0 / D, bias=eps_t[:, 0:1]),
                             reads=[ss, eps_t], writes=[ss], acc=True)
                        P.op("dve", lambda e: e.reciprocal(out=ss[:, 1:2], in_=ss[:, 1:2]), reads=[ss], writes=[ss], acc=True)
                        P.op("dve", lambda e: e.scalar_tensor_tensor(out=h2f[:], in0=x1[:], scalar=ss[:, 1:2], in1=gs2_b[slot][:],
                                                                   op0=ALU.mult, op1=ALU.mult), reads=[x1, ss, gs2_b[slot]], writes=[h2f])
                        P.op("pool", lambda e: e.tensor_tensor(out=h2f[:], in0=h2f[:], in1=sh2_b[slot][:], op=ALU.add),
                             reads=[h2f, sh2_b[slot]], writes=[h2f])
                        P.op("act", lambda e: e.activation(out=h2b[:], in_=h2f[:], func=AF.Copy), reads=[h2f], writes=[h2b])
                        P.dma("sp", lambda e: e.dma_start(out=h2_d[rows, :], in_=h2b[:]), reads=[h2b])
                        P.mm([lambda e, c=c: e.transpose(out=pT32[:, c, :], in_=h2f[:, c * 128:(c + 1) * 128], identity=ident_f[:])
                              for c in range(8)], reads=[h2f, ident_f], writes=[pT32])
                        P.op("act", lambda e: e.activation(out=h2T[:], in_=pT32[:], func=AF.Copy), reads=[pT32], writes=[h2T])
                        P.mm([lambda e, c=c: e.matmul(plg[:, 0:NE], lhsT=h2T[:, c, :], rhs=wr[:, c, :], start=(c == 0), stop=(c == 7))
                              for c in range(8)], reads=[h2T, wr], writes=[plg])
                        P.op("dve", lambda e: e.tensor_tensor(out=lg_all[:, t, :], in0=plg[:, 0:NE], in1=brt[:], op=ALU.add),
                             reads=[plg, brt], writes=[lg_all], acc=(t > 0))
                if "logits" in dbg:
                    lgd = dscr("logits", [T, NE], F32)
                    P.dma("sp", lambda e: e.dma_start(out=lgd[:, :].rearrange("(t p) n -> p t n", p=128), in_=lg_all[:]), reads=[lg_all])
                P.barrier()
            if stop_after == "pD":
                lay.close()
                break

            idx4 = sbuf(lay, "idx4", [128, NT, 4], I32)
            g4 = sbuf(lay, "g4", [128, NT, 4], F32)
            widx = sbuf(lay, "widx", [128, NBLK, 8], I32)
            bidx = sbuf(lay, "bidx", [128, NBLK], I32)
            with ExitStack() as ph:
                m8 = sbuf(ph, "m8", [128, NT, 8], F32)
                mask = sbuf(ph, "mask", [128, NT, NE], BF16)
                gsum = sbuf(ph, "gsum", [128, NT], F32)
                cb = sbuf(ph, "cb", [128, NT, NE], F32)
                sa = sbuf(ph, "sa", [128, NT, NE], F32)
                sb_ = sbuf(ph, "sb_", [128, NT, NE], F32)
                slot = sbuf(ph, "slot", [128, NT, NE], F32)
                oh = sbuf(ph, "oh", [128, NT, NE], F32)
                slot4 = sbuf(ph, "slot4", [128, NT, 4], F32)
                ntot = sbuf(ph, "ntot", [128, NE], F32)
                pad = sbuf(ph, "pad", [128, NE], F32)
                pend = sbuf(ph, "pend", [128, NE], F32)
                pstart = sbuf(ph, "pstart", [128, NE], F32)
                ones32 = sbuf(ph, "ones32", [128, NE], F32)
                jj_i = sbuf(ph, "jj_i", [128, NBLK], I32)
                jj = sbuf(ph, "jj", [128, NBLK], F32)
                cmp = sbuf(ph, "cmp", [128, NBLK, NE], F32)
                cmp2 = sbuf(ph, "cmp2", [128, NE, 16], F32)
                ej = sbuf(ph, "ej", [128, NBLK], F32)
                pc_i = sbuf(ph, "pc_i", [128, 8], I32)
                pc = sbuf(ph, "pc", [128, 8], F32)
                wf = sbuf(ph, "wf", [128, NBLK, 8], F32)
                pcnt = [psum(ph, "pcnt%d" % i, [128, 512], F32) for i in range(4)]
                ppos = [psum(ph, "ppos%d" % i, [128, 512], F32) for i in range(4)]
                for t in range(NT):
                    P.op("dve", lambda e: e.max(out=m8[:, t, :], in_=lg_all[:, t, :]), reads=[lg_all], writes=[m8], acc=(t > 0))
                P.op("dve", lambda e: e.tensor_tensor(out=mask[:], in0=lg_all[:], in1=m8[:, :, 3:4].to_broadcast([128, NT, NE]), op=ALU.is_ge),
                     reads=[lg_all, m8], writes=[mask])
                P.op("dve", lambda e: e.tensor_tensor(out=g4[:], in0=m8[:, :, 0:4], in1=m8[:, :, 0:1].to_broadcast([128, NT, 4]), op=ALU.subtract),
                     reads=[m8], writes=[g4])
                P.op("act", lambda e: e.activation(out=g4[:], in_=g4[:], func=AF.Exp), reads=[g4], writes=[g4])
                P.op("dve", lambda e: e.tensor_reduce(out=gsum[:], in_=g4[:], axis=AX.X, op=ALU.add), reads=[g4], writes=[gsum])
                P.op("dve", lambda e: e.reciprocal(out=gsum[:], in_=gsum[:]), reads=[gsum], writes=[gsum])
                P.op("dve", lambda e: e.tensor_tensor(out=g4[:], in0=g4[:], in1=gsum[:].unsqueeze(2).to_broadcast([128, NT, 4]), op=ALU.mult),
                     reads=[g4, gsum], writes=[g4])
                mflat = mask[:].rearrange("p t e -> p (t e)")
                for q in range(4):
                    P.mm([lambda e: e.matmul(pcnt[q][:], lhsT=ones_bf[:, 0:128], rhs=mflat[:, q * 512:(q + 1) * 512], start=True, stop=True)],
                         reads=[ones_bf, mask], writes=[pcnt[q]])
                    P.mm([lambda e: e.matmul(ppos[q][:], lhsT=ltri[:], rhs=mflat[:, q * 512:(q + 1) * 512], start=True, stop=True)],
                         reads=[ltri, mask], writes=[ppos[q]])
                    P.op("act", lambda e: e.activation(out=cb[:].rearrange("p t e -> p (t e)")[:, q * 512:(q + 1) * 512], in_=pcnt[q][:], func=AF.Copy),
                         reads=[pcnt[q]], writes=[cb], acc=(q > 0))
                P.op("dve", lambda e: e.tensor_copy(out=sa[:], in_=cb[:]), reads=[cb], writes=[sa])
                a, b = sa, sb_
                sh = 1
                while sh < NT:
                    P.op("dve", lambda e: e.tensor_copy(out=b[:, 0:sh, :], in_=a[:, 0:sh, :]), reads=[a], writes=[b])
                    P.op("dve", lambda e: e.tensor_tensor(out=b[:, sh:NT, :], in0=a[:, sh:NT, :], in1=a[:, 0:NT - sh, :], op=ALU.add),
                         reads=[a], writes=[b], acc=True)
                    a, b = b, a
                    sh *= 2
                incl = a
                P.op("dve", lambda e: e.tensor_copy(out=ntot[:], in_=incl[:, NT - 1, :]), reads=[incl], writes=[ntot])
                P.op("pool", lambda e: e.iota(jj_i[:], pattern=[[512, NBLK]], base=0, channel_multiplier=0), writes=[jj_i])
                P.op("pool", lambda e: e.iota(pc_i[:], pattern=[[128, 8]], base=l * NE * D, channel_multiplier=1), writes=[pc_i])
                P.op("dve", lambda e: e.tensor_copy(out=jj[:], in_=jj_i[:]), reads=[jj_i], writes=[jj])
                P.op("dve", lambda e: e.tensor_copy(out=pc[:], in_=pc_i[:]), reads=[pc_i], writes=[pc])
                P.op("dve", lambda e: e.tensor_tensor(out=cmp2[:], in0=ntot[:].unsqueeze(2).to_broadcast([128, NE, 16]),
                                                     in1=jj[:, 0:16].unsqueeze(1).to_broadcast([128, NE, 16]), op=ALU.is_gt),
                     reads=[ntot, jj], writes=[cmp2])
                P.op("dve", lambda e: e.tensor_reduce(out=pad[:], in_=cmp2[:], axis=AX.X, op=ALU.add), reads=[cmp2], writes=[pad])
                P.op("dve", lambda e: e.tensor_scalar(out=pad[:], in0=pad[:], scalar1=512.0, scalar2=None, op0=ALU.mult), reads=[pad], writes=[pad])
                P.op("dve", lambda e: e.memset(ones32[:], 1.0), writes=[ones32])
                P.op("dve", lambda e: e.tensor_tensor_scan(out=pend[:], data0=ones32[:], data1=pad[:], initial=0.0, op0=ALU.mult, op1=ALU.add),
                     reads=[ones32, pad], writes=[pend])
                P.op("dve", lambda e: e.tensor_tensor(out=pstart[:], in0=pend[:], in1=pad[:], op=ALU.subtract), reads=[pend, pad], writes=[pstart])
                P.op("dve", lambda e: e.tensor_tensor(out=b[:], in0=incl[:], in1=cb[:], op=ALU.subtract), reads=[incl, cb], writes=[b])
                P.op("dve", lambda e: e.tensor_tensor(out=b[:], in0=b[:], in1=pstart[:].unsqueeze(1).to_broadcast([128, NT, NE]), op=ALU.add),
                     reads=[b, pstart], writes=[b])
                bflat = b[:].rearrange("p t e -> p (t e)")
                sflat = slot[:].rearrange("p t e -> p (t e)")
                for q in range(4):
                    P.op("dve", lambda e: e.tensor_tensor(out=sflat[:, q * 512:(q + 1) * 512], in0=ppos[q][:], in1=bflat[:, q * 512:(q + 1) * 512], op=ALU.add),
                         reads=[ppos[q], b], writes=[slot], acc=(q > 0))
                for k in range(4):
                    P.op("dve", lambda e: e.tensor_tensor(out=oh[:], in0=lg_all[:], in1=m8[:, :, k:k + 1].to_broadcast([128, NT, NE]), op=ALU.is_equal),
                         reads=[lg_all, m8], writes=[oh])
                    P.op("dve", lambda e: e.tensor_tensor(out=oh[:], in0=oh[:], in1=slot[:], op=ALU.mult), reads=[oh, slot], writes=[oh])
                    P.op("dve", lambda e: e.tensor_reduce(out=slot4[:, :, k], in_=oh[:], axis=AX.X, op=ALU.add), reads=[oh], writes=[slot4], acc=(k > 0))
                P.op("dve", lambda e: e.tensor_copy(out=idx4[:], in_=slot4[:]), reads=[slot4], writes=[idx4])
                P.op("dve", lambda e: e.tensor_tensor(out=cmp[:], in0=pend[:].unsqueeze(1).to_broadcast([128, NBLK, NE]),
                                                     in1=jj[:].unsqueeze(2).to_broadcast([128, NBLK, NE]), op=ALU.is_le),
                     reads=[pend, jj], writes=[cmp])
                P.op("dve", lambda e: e.tensor_reduce(out=ej[:], in_=cmp[:], axis=AX.X, op=ALU.add), reads=[cmp], writes=[ej])
                P.op("dve", lambda e: e.tensor_scalar(out=ej[:], in0=ej[:], scalar1=float(NE - 1), scalar2=None, op0=ALU.min), reads=[ej], writes=[ej])
                P.op("dve", lambda e: e.scalar_tensor_tensor(out=wf[:], in0=ej[:].unsqueeze(2).to_broadcast([128, NBLK, 8]), scalar=float(D),
                                                           in1=pc[:].unsqueeze(1).to_broadcast([128, NBLK, 8]), op0=ALU.mult, op1=ALU.add),
                     reads=[ej, pc], writes=[wf])
                P.op("dve", lambda e: e.tensor_copy(out=widx[:], in_=wf[:]), reads=[wf], writes=[widx])
                P.op("dve", lambda e: e.tensor_scalar(out=ej[:], in0=ej[:], scalar1=float(l * NE), scalar2=None, op0=ALU.add), reads=[ej], writes=[ej])
                P.op("dve", lambda e: e.tensor_copy(out=bidx[:], in_=ej[:]), reads=[ej], writes=[bidx])
                if "idx4" in dbg:
                    for nm, src, shp, dt_ in (("idx4", idx4, [128, NT * 4], I32), ("g4", g4, [128, NT * 4], F32),
                                              ("widx", widx, [128, NBLK * 8], I32), ("bidx", bidx, [128, NBLK], I32)):
                        dd = dscr(nm, shp, dt_)
                        flat = src[:] if len(src.t.shape) == 2 else src[:].rearrange("p a b -> p (a b)")
                        P.dma("sp", lambda e: e.dma_start(out=dd[:, :], in_=flat), reads=[src])
                P.barrier()
            if stop_after == "pE1":
                lay.close()
                break

            with ExitStack() as ph:
                hb_r = Ring([sbuf(ph, "hb%d" % i, [128, D], BF16) for i in range(4)])
                for t in range(NT):
                    hb = hb_r.next()
                    P.dma("sp", lambda e: e.dma_start(out=hb[:], in_=h2_d[t * 128:(t + 1) * 128, :]), writes=[hb])
                    for k in range(4):
                        P.dma("pool", lambda e: e.indirect_dma_start(
                            out=hs_d[:, :], out_offset=bass.IndirectOffsetOnAxis(ap=idx4[:, t, k:k + 1], axis=0),
                            in_=hb[:], in_offset=None), reads=[hb, idx4], sembuf=hb)
                P.barrier()
            if stop_after == "pE2":
                lay.close()
                break

            with ExitStack() as ph:
                wg_r = Ring([sbuf(ph, "wg%d" % i, [128, 8, 2 * D], BF16) for i in range(2)])
                wd_r = Ring([sbuf(ph, "wd%d" % i, [128, 8, D], BF16) for i in range(2)])
                bg_r = Ring([sbuf(ph, "bg%d" % i, [128, 2 * D], BF16) for i in range(2)])
                bdn_r = Ring([sbuf(ph, "bdn%d" % i, [128, D], BF16) for i in range(2)])
                hsT_r = Ring([sbuf(ph, "hsT%d" % i, [128, 8, BLK], BF16) for i in range(2)])
                actT_r = Ring([sbuf(ph, "actT%d" % i, [128, 8, BLK], BF16) for i in range(2)])
                sig_r = Ring([sbuf(ph, "sig%d" % i, [128, BLK], F32) for i in range(2)])
                t1_r = Ring([sbuf(ph, "et1_%d" % i, [128, BLK], F32) for i in range(2)])
                uc_r = Ring([sbuf(ph, "uc%d" % i, [128, BLK], F32) for i in range(2)])
                yo_r = Ring([sbuf(ph, "yo%d" % i, [128, D], BF16) for i in range(3)])
                pg_r = Ring([psum(ph, "pg%d" % i, [128, 512], F32) for i in range(2)])
                pu_r = Ring([psum(ph, "pue%d" % i, [128, 512], F32) for i in range(2)])
                po_r = Ring([psum(ph, "poe%d" % i, [128, 512], F32) for i in range(3)])

                def load_block(j):
                    wg = wg_r.next(); wd = wd_r.next(); bg = bg_r.next(); bdn = bdn_r.next(); hsT = hsT_r.next()
                    for c in range(8):
                        P.dma("sp", lambda e: e.dma_start_transpose(out=hsT[:, c, :], in_=hs_d[j * BLK:(j + 1) * BLK, c * 128:(c + 1) * 128]),
                              writes=[hsT], acc=(c > 0))
                    for c in range(8):
                        P.dma("pool", lambda e: e.indirect_dma_start(
                            out=wg[:, c, :], out_offset=None, in_=w_gu[:, :],
                            in_offset=bass.IndirectOffsetOnAxis(ap=widx[:, j, c:c + 1], axis=0)), reads=[widx], writes=[wg], acc=(c > 0))
                    P.dma("pool", lambda e: e.indirect_dma_start(
                        out=bg[:], out_offset=None, in_=b_gu[:, :],
                        in_offset=bass.IndirectOffsetOnAxis(ap=bidx[:, j:j + 1], axis=0)), reads=[bidx], writes=[bg])
                    for c in range(8):
                        P.dma("pool", lambda e: e.indirect_dma_start(
                            out=wd[:, c, :], out_offset=None, in_=w_dn[:, :],
                            in_offset=bass.IndirectOffsetOnAxis(ap=widx[:, j, c:c + 1], axis=0)), reads=[widx], writes=[wd], acc=(c > 0))
                    P.dma("pool", lambda e: e.indirect_dma_start(
                        out=bdn[:], out_offset=None, in_=b_dn[:, :],
                        in_offset=bass.IndirectOffsetOnAxis(ap=bidx[:, j:j + 1], axis=0)), reads=[bidx], writes=[bdn])
                    return wg, wd, bg, bdn, hsT

                nblk_run = NBLK
                nxt = load_block(0)
                for j in range(nblk_run):
                    wg, wd, bg, bdn, hsT = nxt
                    if j + 1 < nblk_run:
                        nxt = load_block(j + 1)
                    actT = actT_r.next()
                    for fo in range(8):
                        pg = pg_r.next(); pu = pu_r.next()
                        gsl = slice(fo * 256, (fo + 1) * 256, 2)
                        usl = slice(fo * 256 + 1, (fo + 1) * 256, 2)
                        P.mm([lambda e, c=c: e.matmul(pg[:], lhsT=wg[:, c, gsl], rhs=hsT[:, c, :], start=(c == 0), stop=False) for c in range(8)]
                             + [lambda e: e.matmul(pg[:], lhsT=bg[0:1, gsl], rhs=ones_bf[0:1, 0:BLK], start=False, stop=True)],
                             reads=[wg, hsT, bg, ones_bf], writes=[pg])
                        P.mm([lambda e, c=c: e.matmul(pu[:], lhsT=wg[:, c, usl], rhs=hsT[:, c, :], start=(c == 0), stop=False) for c in range(8)]
                             + [lambda e: e.matmul(pu[:], lhsT=bg[0:1, usl], rhs=ones_bf[0:1, 0:BLK], start=False, stop=True)],
                             reads=[wg, hsT, bg, ones_bf], writes=[pu])
                        sig = sig_r.next(); t1 = t1_r.next(); uc = uc_r.next()
                        P.op("act", lambda e: e.activation(out=sig[:], in_=pg[:], func=AF.Sigmoid, scale=1.702), reads=[pg], writes=[sig])
                        P.op("dve", lambda e: e.scalar_tensor_tensor(out=t1[:], in0=pg[:], scalar=7.0, in1=sig[:], op0=ALU.min, op1=ALU.mult),
                             reads=[pg, sig], writes=[t1])
                        P.op("dve", lambda e: e.tensor_scalar(out=uc[:], in0=pu[:], scalar1=-7.0, scalar2=7.0, op0=ALU.max, op1=ALU.min),
                             reads=[pu], writes=[uc])
                        P.op("dve", lambda e: e.scalar_tensor_tensor(out=actT[:, fo, :], in0=uc[:], scalar=1.0, in1=t1[:], op0=ALU.add, op1=ALU.mult),
                             reads=[uc, t1], writes=[actT], acc=(fo > 0))
                    for ts in range(4):
                        yo = yo_r.next()
                        for half in range(2):
                            hsl = slice(half * 512, (half + 1) * 512)
                            po = po_r.next()
                            P.mm([lambda e, fo=fo: e.matmul(po[:], lhsT=actT[:, fo, ts * 128:(ts + 1) * 128], rhs=wd[:, fo, hsl],
                                                            start=(fo == 0), stop=False) for fo in range(8)]
                                 + [lambda e: e.matmul(po[:], lhsT=ones_bf[0:1, 0:128], rhs=bdn[0:1, hsl], start=False, stop=True)],
                                 reads=[actT, wd, bdn, ones_bf], writes=[po])
                            if half == 0:
                                P.op("act", lambda e: e.activation(out=yo[:, hsl], in_=po[:], func=AF.Copy), reads=[po], writes=[yo])
                            else:
                                P.op("dve", lambda e: e.tensor_copy(out=yo[:, hsl], in_=po[:]), reads=[po], writes=[yo], acc=True)
                        P.dma("sp", lambda e: e.dma_start(out=ys_d[j * BLK + ts * 128:j * BLK + (ts + 1) * 128, :], in_=yo[:]), reads=[yo])
                P.barrier()
            if stop_after == "pE3":
                lay.close()
                break

            with ExitStack() as ph:
                ga2_b = [sbuf(ph, "ga2_b%d" % i, [128, D], F32) for i in range(2)]
                gfin_b = sbuf(ph, "gfin_b", [128, D], F32)
                for s_ in range(2):
                    P.dma("sp", lambda e: e.dma_start(out=ga2_b[s_][:], in_=modd[s_:s_ + 1, 5 * D:6 * D].partition_broadcast(128)), writes=[ga2_b[s_]])
                P.dma("sp", lambda e: e.dma_start(out=gfin_b[:], in_=g_final[0:1, :].partition_broadcast(128)), writes=[gfin_b])
                xm_r = Ring([sbuf(ph, "xm%d" % i, [128, D], F32) for i in range(3)])
                yg_r = Ring([sbuf(ph, "yg%d" % i, [128, 4, D], BF16) for i in range(3)])
                ac_r = Ring([sbuf(ph, "ac%d" % i, [128, D], F32) for i in range(2)])
                x2_r = Ring([sbuf(ph, "x2_%d" % i, [128, D], F32) for i in range(3)])
                sqf = sbuf(ph, "sqf", [128, D], BF16)
                ssf_r = Ring([sbuf(ph, "ssf%d" % i, [128, 2], F32) for i in range(2)])
                x_dst = y_out if last else xres

                def load_tile(t):
                    xm = xm_r.next(); yg = yg_r.next()
                    P.dma("sp", lambda e: e.dma_start(out=xm[:], in_=xmid[t * 128:(t + 1) * 128, :]), writes=[xm])
                    for k in range(4):
                        P.dma("pool", lambda e: e.indirect_dma_start(
                            out=yg[:, k, :], out_offset=None, in_=ys_d[:, :],
                            in_offset=bass.IndirectOffsetOnAxis(ap=idx4[:, t, k:k + 1], axis=0)), reads=[idx4], writes=[yg], acc=(k > 0))
                    return xm, yg

                nxt = load_tile(0)
                for t in range(NT):
                    slot_ = t // 32
                    xm, yg = nxt
                    if t + 1 < NT:
                        nxt = load_tile(t + 1)
                    ac = ac_r.next(); x2 = x2_r.next()
                    P.op("dve", lambda e: e.tensor_scalar(out=ac[:], in0=yg[:, 0, :], scalar1=g4[:, t, 0:1], scalar2=None, op0=ALU.mult),
                         reads=[yg, g4], writes=[ac])
                    for k in range(1, 4):
                        P.op("dve", lambda e: e.scalar_tensor_tensor(out=ac[:], in0=yg[:, k, :], scalar=g4[:, t, k:k + 1], in1=ac[:],
                                                                   op0=ALU.mult, op1=ALU.add), reads=[yg, g4, ac], writes=[ac])
                    P.op("pool", lambda e: e.tensor_tensor(out=ac[:], in0=ac[:], in1=ga2_b[slot_][:], op=ALU.mult), reads=[ac, ga2_b[slot_]], writes=[ac])
                    P.op("pool", lambda e: e.tensor_tensor(out=x2[:], in0=ac[:], in1=xm[:], op=ALU.add), reads=[ac, xm], writes=[x2])
                    if last:
                        ss = ssf_r.next()
                        P.op("act", lambda e: e.activation(out=sqf[:], in_=x2[:], func=AF.Square, accum_out=ss[:, 0:1]), reads=[x2], writes=[sqf, ss])
                        P.op("act", lambda e: e.activation(out=ss[:, 1:2], in_=ss[:, 0:1], func=AF.Sqrt, scale=1.0 / D, bias=eps_t[:, 0:1]),
                             reads=[ss, eps_t], writes=[ss], acc=True)
                        P.op("dve", lambda e: e.reciprocal(out=ss[:, 1:2], in_=ss[:, 1:2]), reads=[ss], writes=[ss], acc=True)
                        P.op("dve", lambda e: e.scalar_tensor_tensor(out=x2[:], in0=x2[:], scalar=ss[:, 1:2], in1=gfin_b[:],
                                                                   op0=ALU.mult, op1=ALU.mult), reads=[x2, ss, gfin_b], writes=[x2])
                    P.dma("sp", lambda e: e.dma_start(out=x_dst[t * 128:(t + 1) * 128, :], in_=x2[:]), reads=[x2])
                P.barrier()
            lay.close()
        P.barrier()
    return nc


_CONST_CACHE = {}


def _consts(kind):
    if kind in _CONST_CACHE:
        return _CONST_CACHE[kind]
    S = 4096 if kind == "prompt" else 8192
    bf16 = ml_dtypes.bfloat16
    pos = (np.arange(T) % S).astype(np.float32)
    inv = (1.0 / (10000.0 ** (np.arange(0, 32, 2, dtype=np.float32) / 32.0))).astype(np.float32)
    ang = pos[:, None] * inv[None, :]
    cos = np.cos(ang).astype(np.float32).T
    sin = np.sin(ang).astype(np.float32).T
    c2 = np.concatenate([cos, cos], 0)
    s2 = np.concatenate([sin, sin], 0)
    sc = np.float32(96.0 ** -0.5)
    rope = np.stack([c2 * sc, s2 * sc, c2, s2], 0).astype(np.float32)
    maskb = np.zeros((128, 4), np.float32)
    if kind == "prompt":
        maskb[:, 1] = -30000.0
        maskb[:, 2] = -30000.0
    n = np.arange(T, dtype=np.int64)
    fcs = np.empty((2, NG, 8, 128, 8 * 512), dtype=bf16)
    scale = 1.0 / np.sqrt(S * 64.0)
    seq_n = n // S
    for kc in range(NG):
        k = np.arange(kc * 512, (kc + 1) * 512, dtype=np.int64)
        prod = ((n % S)[:, None] * (k % S)[None, :]) % S
        angk = prod.astype(np.float64) * (2.0 * np.pi / S)
        same = (seq_n[:, None] == (k // S)[None, :])
        fc = np.where(same, np.cos(angk) * scale, 0.0).astype(np.float32)
        fs = np.where(same, np.sin(angk) * scale, 0.0).astype(np.float32)
        for i, f in enumerate((fc, fs)):
            f = f.reshape(8, 8, 128, 512).transpose(0, 2, 1, 3).reshape(8, 128, 8 * 512)
            fcs[i, kc] = f.astype(bf16)
    cj = np.arange(64)
    a64 = 2.0 * np.pi * np.outer(cj, cj) / 64.0
    bd = np.zeros((2, 128, 128), np.float32)
    for b in range(2):
        bd[0, b * 64:(b + 1) * 64, b * 64:(b + 1) * 64] = np.cos(a64)
        bd[1, b * 64:(b + 1) * 64, b * 64:(b + 1) * 64] = -np.sin(a64)
    out = dict(rope=rope, maskb=maskb, fcs=fcs, bd=bd.astype(bf16),
               ident_bf=np.eye(128, dtype=np.float32).astype(bf16), ident_f=np.eye(128, dtype=np.float32),
               ltri=np.triu(np.ones((128, 128), np.float32), 1).astype(bf16))
    _CONST_CACHE[kind] = out
    return out


def make_in_maps(inputs, depth, cores=range(8)):
    f = lambda a: np.ascontiguousarray(np.asarray(a))
    xp, xs = f(inputs["x_prompt"]), f(inputs["x_sample"])
    cp, cs = f(inputs["c_prompt"]), f(inputs["c_sample"])
    shared = {}
    for k in ("w_ada", "b_ada", "g_mix", "g_ffn", "w_in", "g_q", "w_uq", "g_kv", "w_ukv", "w_a", "w_b", "w_out",
              "w_router", "b_router"):
        shared[k] = f(inputs[k])[:depth]
    shared["w_gu"] = f(inputs["w_gu"])[:depth].reshape(depth * NE * D, 2 * D)
    shared["b_gu"] = f(inputs["b_gu"])[:depth].reshape(depth * NE, 2 * D)
    shared["w_dn"] = f(inputs["w_dn"])[:depth].reshape(depth * NE * D, D)
    shared["b_dn"] = f(inputs["b_dn"])[:depth].reshape(depth * NE, D)
    shared["g_final"] = f(inputs["g_final"]).reshape(1, D)
    maps = []
    for c in cores:
        m = dict(shared)
        if c < 4:
            m["x"] = xp[2 * c:2 * c + 2].reshape(T, D)
            m["c2"] = cp[2 * c:2 * c + 2]
            m.update(_consts("prompt"))
        else:
            m["x"] = xs[c - 4].reshape(T, D)
            m["c2"] = np.stack([cs[c - 4], cs[c - 4]], 0)
            m.update(_consts("sample"))
        maps.append(m)
    return maps


_NC_CACHE = {}


def kernel(**inputs):
    depth = 4
    if depth not in _NC_CACHE:
        _NC_CACHE[depth] = build_program(depth)
    nc = _NC_CACHE[depth]
    maps = make_in_maps(inputs, depth)
    res = run_bass_kernel_spmd(nc, maps, core_ids=list(range(8)))
    ys = [np.asarray(r["y"], dtype=np.float32) for r in res.results]
    y_prompt = np.stack([ys[c].reshape(2, 4096, D) for c in range(4)], 0).reshape(8, 4096, D)
    y_sample = np.stack([ys[c].reshape(T, D) for c in range(4, 8)], 0)
    return (y_prompt, y_sample)
```

```python
import numpy as np
import ml_dtypes
from contextlib import ExitStack
import concourse.bass as bass
import concourse.mybir as mybir
from concourse.bass_utils import run_bass_kernel_spmd

F32 = mybir.dt.float32
BF16 = mybir.dt.bfloat16
I32 = mybir.dt.int32
AF = mybir.ActivationFunctionType
ALU = mybir.AluOpType
AX = mybir.AxisListType

T = 8192
D = 1024
NT = 64
NG = 16
H = 8
O1, O2, O3, O4, O5, INW = 384, 640, 672, 1184, 2208, 3232
NE = 32
BLK = 512
NBLK = 96
NSLOT = NBLK * BLK
EPS = 1e-6
ENGS = ("pe", "act", "dve", "pool", "sp")


class Buf:
    __slots__ = ("t", "w", "r", "sem", "excl")

    def __init__(self, t, excl=False):
        self.t = t
        self.excl = excl
        self.w = {}
        self.r = {}
        self.sem = None

    def __getitem__(self, k):
        return self.t[k]


class Ring:
    def __init__(self, bufs):
        self.b = list(bufs)
        self.i = -1

    def next(self):
        self.i = (self.i + 1) % len(self.b)
        return self.b[self.i]


class Prog:
    def __init__(self, nc, st):
        self.nc = nc
        self.st = st
        self.h = {"pe": nc.tensor, "act": nc.scalar, "dve": nc.vector, "pool": nc.gpsimd, "sp": nc.sync}
        self.sems = {e: st.enter_context(nc.semaphore("s_" + e)) for e in ENGS}
        self.cnt = {e: 0 for e in ENGS}
        self.waited = {e: {} for e in ENGS}
        self.dcnt = {}
        self.n_inst = 0
        self.free_sems = []
        self.sem_bufs = []

    def new_sem(self):
        if self.free_sems:
            return self.free_sems.pop()
        k = ("d", len(self.dcnt))
        self.sems[k] = self.st.enter_context(self.nc.semaphore("d_%d" % k[1]))
        self.dcnt[k] = 0
        return k

    def _wait(self, eng, key, val):
        if self.waited[eng].get(key, 0) >= val:
            return
        self.waited[eng][key] = val
        self.h[eng].wait_ge(self.sems[key], val)
        self.n_inst += 1

    def _deps(self, eng, reads, writes, acc, deps):
        need = {}
        for k, v in deps:
            need[k] = max(need.get(k, 0), v)
        for b in reads:
            for k, v in b.w.items():
                need[k] = max(need.get(k, 0), v)
            if b.excl:
                for k, v in b.r.items():
                    if k != eng:
                        need[k] = max(need.get(k, 0), v)
        for b in writes:
            for k, v in b.r.items():
                if k != eng:
                    need[k] = max(need.get(k, 0), v)
            if not acc:
                for k, v in b.w.items():
                    if k != eng:
                        need[k] = max(need.get(k, 0), v)
        for k, v in need.items():
            self._wait(eng, k, v)

    def _record(self, ev, reads, writes, acc):
        k, v = ev
        for b in reads:
            b.r[k] = v
        for b in writes:
            if acc:
                b.w[k] = v
            else:
                b.w = {k: v}
                b.r = {}

    def op(self, eng, fn, reads=(), writes=(), acc=False, deps=()):
        self._deps(eng, reads, writes, acc, deps)
        ins = fn(self.h[eng])
        self.n_inst += 1
        self.cnt[eng] += 1
        ins.then_inc(self.sems[eng], 1)
        ev = (eng, self.cnt[eng])
        self._record(ev, reads, writes, acc)
        return ev

    def mm(self, fns, reads=(), writes=(), acc=False, deps=()):
        self._deps("pe", reads, writes, acc, deps)
        pe = self.h["pe"]
        ins = None
        for fn in fns:
            ins = fn(pe)
            self.n_inst += 1
        self.cnt["pe"] += 1
        ins.then_inc(self.sems["pe"], 1)
        ev = ("pe", self.cnt["pe"])
        self._record(ev, reads, writes, acc)
        return ev

    def dma(self, eng, fn, reads=(), writes=(), acc=False, deps=(), sembuf=None):
        self._deps(eng, reads, writes, acc, deps)
        sb = sembuf if sembuf is not None else (writes[0] if writes else reads[0])
        if sb.sem is None:
            sb.sem = self.new_sem()
            self.sem_bufs.append(sb)
        k = sb.sem
        self.dcnt[k] += 16
        fn(self.h[eng]).then_inc(self.sems[k], 16)
        self.n_inst += 1
        ev = (k, self.dcnt[k])
        self._record(ev, reads, writes, acc)
        return ev

    def barrier(self):
        for k, v in self.dcnt.items():
            self._wait("sp", k, v)
        for e in ENGS:
            if e != "sp":
                self._wait("sp", e, self.cnt[e])
        self.cnt["sp"] += 1
        self.h["sp"].nop().then_inc(self.sems["sp"], 1)
        for e in ENGS:
            if e != "sp":
                self._wait(e, "sp", self.cnt["sp"])
        for e in ENGS:
            for k, v in self.dcnt.items():
                self.waited[e][k] = v
            for e2 in ENGS:
                self.waited[e][e2] = self.cnt[e2]
        for b in self.sem_bufs:
            self.free_sems.append(b.sem)
            b.sem = None
        self.sem_bufs = []


def build_program(depth, stop_after=None, dbg=()):
    nc = bass.Bass("TRN2", target_bir_lowering=False)

    def din(name, shape, dt):
        return nc.dram_tensor(name, list(shape), dt, kind="ExternalInput").ap()

    def dscr(name, shape, dt):
        kind = "ExternalOutput" if name in dbg else "Internal"
        return nc.dram_tensor(name, list(shape), dt, kind=kind).ap()

    L = depth
    x_in = din("x", [T, D], F32)
    c_in = din("c2", [2, D], F32)
    w_ada = din("w_ada", [L, D, 6 * D], F32)
    b_ada = din("b_ada", [L, 6 * D], F32)
    g_mix = din("g_mix", [L, D], F32)
    g_ffn = din("g_ffn", [L, D], F32)
    w_in = din("w_in", [L, D, INW], F32)
    g_q = din("g_q", [L, 384], F32)
    w_uq = din("w_uq", [L, 384, 768], F32)
    g_kv = din("g_kv", [L, 256], F32)
    w_ukv = din("w_ukv", [L, 256, 1024], F32)
    w_a = din("w_a", [L, 512, D], F32)
    w_b = din("w_b", [L, 512, D], F32)
    w_out = din("w_out", [L, D, D], F32)
    w_router = din("w_router", [L, D, NE], F32)
    b_router = din("b_router", [L, NE], F32)
    w_gu = din("w_gu", [L * NE * D, 2 * D], F32)
    b_gu = din("b_gu", [L * NE, 2 * D], F32)
    w_dn = din("w_dn", [L * NE * D, D], F32)
    b_dn = din("b_dn", [L * NE, D], F32)
    g_final = din("g_final", [1, D], F32)
    rope = din("rope", [4, 32, T], F32)
    maskb = din("maskb", [128, 4], F32)
    fcs = din("fcs", [2, NG, 8, 128, 8 * 512], BF16)
    ident_bf_d = din("ident_bf", [128, 128], BF16)
    ident_f_d = din("ident_f", [128, 128], F32)
    ltri_d = din("ltri", [128, 128], BF16)
    bd_d = din("bd", [2, 128, 128], BF16)
    y_out = nc.dram_tensor("y", [T, D], F32, kind="ExternalOutput").ap()

    xmid = dscr("xmid", [T, D], F32)
    xres = dscr("xres", [T, D], F32)
    modd = dscr("modd", [2, 6 * D], F32)
    qT_d = dscr("qT", [H, 96, T], BF16)
    knT_d = dscr("knT", [H, 64, T], BF16)
    krT_d = dscr("krT", [32, T], BF16)
    v_d = dscr("v", [T, 512], BF16)
    uf_d = dscr("uf", [T, 512], BF16)
    gT_d = dscr("gT", [16, 128, T], BF16)
    aoT_d = dscr("aoT", [512, T], BF16)
    yfT_d = dscr("yfT", [2, 512, T], BF16)
    h2_d = dscr("h2", [T, D], BF16)
    hs_d = dscr("hs", [NSLOT, D], BF16)
    ys_d = dscr("ys", [NSLOT, D], BF16)

    with ExitStack() as st:
        P = Prog(nc, st)

        uid = [0]

        def sbuf(ctx, name, shape, dt):
            uid[0] += 1
            return Buf(ctx.enter_context(nc.sbuf_tensor("sb%d_%s" % (uid[0], name), list(shape), dt)))

        def psum(ctx, name, shape, dt):
            uid[0] += 1
            return Buf(ctx.enter_context(nc.psum_tensor("ps%d_%s" % (uid[0], name), list(shape), dt)), excl=True)

        ident_bf = sbuf(st, "ident_bf", [128, 128], BF16)
        ident_f = sbuf(st, "ident_f", [128, 128], F32)
        ltri = sbuf(st, "ltri", [128, 128], BF16)
        ones_bf = sbuf(st, "ones_bf", [128, 512], BF16)
        ones_f = sbuf(st, "ones_f", [128, 128], F32)
        eps_t = sbuf(st, "eps_t", [128, 1], F32)
        mb = sbuf(st, "mb", [128, 4], F32)
        scT = sbuf(st, "scT", [128, 8, 2], F32)
        P.dma("sp", lambda e: e.dma_start(out=ident_bf[:], in_=ident_bf_d[:, :]), writes=[ident_bf])
        P.dma("sp", lambda e: e.dma_start(out=ident_f[:], in_=ident_f_d[:, :]), writes=[ident_f])
        P.dma("sp", lambda e: e.dma_start(out=ltri[:], in_=ltri_d[:, :]), writes=[ltri])
        P.dma("sp", lambda e: e.dma_start(out=mb[:], in_=maskb[:, :]), writes=[mb])
        P.op("dve", lambda e: e.memset(ones_bf[:], 1.0), writes=[ones_bf])
        P.op("dve", lambda e: e.memset(ones_f[:], 1.0), writes=[ones_f])
        P.op("dve", lambda e: e.memset(eps_t[:], EPS), writes=[eps_t])
        for s in range(2):
            P.dma("sp", lambda e, s=s: e.dma_start(
                out=scT[:, :, s], in_=c_in[s, :].rearrange("(j p) -> p j", p=128),
                allow_slow_non_contiguous=True), writes=[scT], acc=True)
        P.op("act", lambda e: e.activation(out=scT[:], in_=scT[:], func=AF.Silu), reads=[scT], writes=[scT])
        modT = sbuf(st, "modT", [128, 48, 2], F32)
        gs1T = sbuf(st, "gs1T", [128, 8, 2], F32)
        gmT = sbuf(st, "gmT", [128, 8], F32)
        gqT = sbuf(st, "gqT", [128, 3], F32)
        gkvT = sbuf(st, "gkvT", [128, 2], F32)
        P.barrier()

        def cast_load(dst, dst_ap_fn, src_rows, ncols, nchunk, col0=0, writes_acc=False):
            first = True
            for c in range(nchunk):
                for cc in range(0, ncols, 2048):
                    w = min(2048, ncols - cc)
                    P.dma("pool", lambda e, c=c, cc=cc, w=w: e.dma_start(
                        out=dst_ap_fn(c, cc, w), in_=src_rows[c * 128:(c + 1) * 128, col0 + cc:col0 + cc + w]),
                        writes=[dst], acc=(not first) or writes_acc)
                    first = False

        for l in range(L):
            x_src = x_in if l == 0 else xres
            last = (l == L - 1)
            with ExitStack() as ph:
                ba2 = sbuf(ph, "ba2", [2, 6 * D], F32)
                mod_sb = sbuf(ph, "mod_sb", [2, 6 * D], F32)
                wa_r = Ring([sbuf(ph, "wa%d" % i, [128, 8, 512], F32) for i in range(2)])
                pm_r = Ring([psum(ph, "pm%d" % i, [2, 512], F32) for i in range(2)])
                P.dma("sp", lambda e: e.dma_start(out=ba2[:], in_=b_ada[l:l + 1, :].partition_broadcast(2)), writes=[ba2])
                for ng in range(12):
                    wa = wa_r.next()
                    pm = pm_r.next()
                    P.dma("sp", lambda e: e.dma_start(
                        out=wa[:], in_=w_ada[l, :, ng * 512:(ng + 1) * 512].rearrange("(j p) n -> p j n", p=128)),
                        writes=[wa])
                    P.mm([lambda e, j=j: e.matmul(pm[:], lhsT=scT[:, j, :], rhs=wa[:, j, :], start=(j == 0), stop=(j == 7))
                          for j in range(8)], reads=[scT, wa], writes=[pm])
                    P.op("dve", lambda e: e.tensor_tensor(out=mod_sb[:, ng * 512:(ng + 1) * 512], in0=pm[:],
                                                         in1=ba2[:, ng * 512:(ng + 1) * 512], op=ALU.add),
                         reads=[pm, ba2], writes=[mod_sb], acc=(ng > 0))
                P.dma("sp", lambda e: e.dma_start(out=modd[:, :], in_=mod_sb[:]), reads=[mod_sb])
                P.barrier()
                for s in range(2):
                    P.dma("sp", lambda e, s=s: e.dma_start(
                        out=modT[:, :, s], in_=modd[s, :].rearrange("(j p) -> p j", p=128),
                        allow_slow_non_contiguous=True), writes=[modT], acc=(s > 0))
                P.dma("sp", lambda e: e.dma_start(out=gmT[:], in_=g_mix[l, :].rearrange("(j p) -> p j", p=128),
                                                  allow_slow_non_contiguous=True), writes=[gmT])
                P.dma("sp", lambda e: e.dma_start(out=gqT[:], in_=g_q[l, :].rearrange("(j p) -> p j", p=128),
                                                  allow_slow_non_contiguous=True), writes=[gqT])
                P.dma("sp", lambda e: e.dma_start(out=gkvT[:], in_=g_kv[l, :].rearrange("(j p) -> p j", p=128),
                                                  allow_slow_non_contiguous=True), writes=[gkvT])
                for s in range(2):
                    P.op("dve", lambda e, s=s: e.scalar_tensor_tensor(out=gs1T[:, :, s], in0=modT[:, 8:16, s], scalar=1.0,
                                                                     in1=gmT[:], op0=ALU.add, op1=ALU.mult),
                         reads=[modT, gmT], writes=[gs1T], acc=(s > 0))
                P.barrier()
            if stop_after == "p0":
                break

            with ExitStack() as ph:
                win = sbuf(ph, "win", [128, 8, INW], BF16)
                wuq = sbuf(ph, "wuq", [128, 3, 768], BF16)
                wuqr = sbuf(ph, "wuqr", [128, 3, 768], BF16)
                wukv = sbuf(ph, "wukv", [128, 2, 1024], BF16)
                wkr = sbuf(ph, "wkr", [128, 8, 96], BF16)
                wkrr = sbuf(ph, "wkrr", [128, 8, 96], BF16)
                cast_load(win, lambda c, cc, w: win[:, c, cc:cc + w], w_in[l], INW, 8)
                cast_load(wuq, lambda c, cc, w: wuq[:, c, cc:cc + w], w_uq[l], 768, 3)
                cast_load(wukv, lambda c, cc, w: wukv[:, c, cc:cc + w], w_ukv[l], 1024, 2)
                P.op("pool", lambda e: e.memset(wuqr[:], 0.0), writes=[wuqr])
                P.op("pool", lambda e: e.memset(wkr[:], 0.0), writes=[wkr])
                P.op("pool", lambda e: e.memset(wkrr[:], 0.0), writes=[wkrr])
                wuq4 = lambda t: t[:].rearrange("p c (h r) -> p c h r", h=8)
                P.op("dve", lambda e: e.tensor_scalar(out=wuq4(wuqr)[:, :, :, 64:80], in0=wuq4(wuq)[:, :, :, 80:96],
                                                     scalar1=-1.0, scalar2=None, op0=ALU.mult),
                     reads=[wuq], writes=[wuqr], acc=True, deps=list(wuqr.w.items()))
                P.op("dve", lambda e: e.tensor_copy(out=wuq4(wuqr)[:, :, :, 80:96], in_=wuq4(wuq)[:, :, :, 64:80]),
                     reads=[wuq], writes=[wuqr], acc=True)
                P.op("dve", lambda e: e.tensor_copy(out=wkr[:, :, 64:96], in_=win[:, :, O2:O3]),
                     reads=[win], writes=[wkr], acc=True, deps=list(wkr.w.items()))
                P.op("dve", lambda e: e.tensor_scalar(out=wkrr[:, :, 64:80], in0=win[:, :, O2 + 16:O3],
                                                     scalar1=-1.0, scalar2=None, op0=ALU.mult),
                     reads=[win], writes=[wkrr], acc=True, deps=list(wkrr.w.items()))
                P.op("dve", lambda e: e.tensor_copy(out=wkrr[:, :, 80:96], in_=win[:, :, O2:O2 + 16]),
                     reads=[win], writes=[wkrr], acc=True)

                x_r = Ring([sbuf(ph, "xt%d" % i, [128, D], F32) for i in range(2)])
                sq_scr = sbuf(ph, "sq_scr", [128, D], BF16)
                ss_r = Ring([sbuf(ph, "ss%d" % i, [128, 2], F32) for i in range(3)])
                xn_r = Ring([sbuf(ph, "xn%d" % i, [128, D], BF16) for i in range(4)])
                pT_r = Ring([psum(ph, "pT%d" % i, [128, 4, 512], BF16) for i in range(1)])
                hT_r = Ring([sbuf(ph, "hT%d" % i, [128, 8, 512], BF16) for i in range(2)])
                pu_r = Ring([psum(ph, "pu%d" % i, [128, 512], F32) for i in range(4)])
                pq_r = Ring([psum(ph, "pq%d" % i, [128, 512], F32) for i in range(2)])
                uq_sb = sbuf(ph, "uq_sb", [128, 5, 512], F32)
                sq_sb = sbuf(ph, "sq_sb", [128, 5, 512], BF16)
                rs_r = Ring([sbuf(ph, "rs%d" % i, [128, 512], F32) for i in range(2)])
                qn = sbuf(ph, "qn", [128, 5, 512], BF16)
                qT_sb_r = Ring([sbuf(ph, "qT_sb%d" % i, [96, 8, 512], BF16) for i in range(1)])
                knT_sb_r = Ring([sbuf(ph, "knT_sb%d" % i, [64, 8, 512], BF16) for i in range(1)])
                krT_sb_r = Ring([sbuf(ph, "krT_sb%d" % i, [96, 512], BF16) for i in range(2)])
                rt_r = Ring([sbuf(ph, "rt%d" % i, [96, 2, 512], F32) for i in range(2)])
                v_sb_r = Ring([sbuf(ph, "v_sb%d" % i, [128, 512], BF16) for i in range(2)])
                f_sb_r = Ring([sbuf(ph, "f_sb%d" % i, [128, 512], BF16) for i in range(2)])
                g_sb_r = Ring([sbuf(ph, "g_sb%d" % i, [128, 512], BF16) for i in range(3)])
                tab_r = Ring([sbuf(ph, "tab%d" % i, [96, 4, 512], F32) for i in range(1)])

                for g in range(NG):
                    slot = g // 8
                    cols = slice(g * 512, (g + 1) * 512)
                    tab = tab_r.next()
                    P.dma("sp", lambda e: e.dma_start(out=tab[64:96, :, :], in_=rope[:, :, cols].rearrange("t p n -> p t n")),
                          writes=[tab])
                    hT = hT_r.next()
                    for half in range(2):
                        pT = pT_r.next()
                        if half == 0:
                            xns = []
                            for ti in range(4):
                                t = g * 4 + ti
                                xt = x_r.next(); ss = ss_r.next(); xn = xn_r.next()
                                P.dma("sp", lambda e: e.dma_start(out=xt[:], in_=x_src[t * 128:(t + 1) * 128, :]), writes=[xt])
                                P.op("act", lambda e: e.activation(out=sq_scr[:], in_=xt[:], func=AF.Square, accum_out=ss[:, 0:1]),
                                     reads=[xt], writes=[sq_scr, ss])
                                P.op("act", lambda e: e.activation(out=ss[:, 1:2], in_=ss[:, 0:1], func=AF.Sqrt, scale=1.0 / D, bias=eps_t[:, 0:1]),
                                     reads=[ss, eps_t], writes=[ss], acc=True)
                                P.op("dve", lambda e: e.reciprocal(out=ss[:, 1:2], in_=ss[:, 1:2]), reads=[ss], writes=[ss], acc=True)
                                P.op("dve", lambda e: e.tensor_scalar(out=xn[:], in0=xt[:], scalar1=ss[:, 1:2], scalar2=None, op0=ALU.mult),
                                     reads=[xt, ss], writes=[xn])
                                xns.append(xn)
                        first = True
                        for cj in range(4):
                            c = half * 4 + cj
                            for ti in range(4):
                                P.mm([lambda e: e.transpose(out=pT[:, cj, ti * 128:(ti + 1) * 128],
                                                            in_=xns[ti][:, c * 128:(c + 1) * 128], identity=ident_bf[:])],
                                     reads=[xns[ti], ident_bf], writes=[pT], acc=not first)
                                first = False
                        for cj in range(4):
                            c = half * 4 + cj
                            P.op("act", lambda e: e.activation(out=hT[:, c, :], in_=pT[:, cj, :], func=AF.Identity,
                                                               scale=gs1T[:, c, slot:slot + 1], bias=modT[:, c, slot:slot + 1]),
                                 reads=[pT, gs1T, modT], writes=[hT], acc=not (half == 0 and cj == 0))

                    if stop_after == "pA.hT":
                        break
                    def inproj(col0, m, lhs=None):
                        pu = pu_r.next()
                        wsrc = win if lhs is None else lhs
                        P.mm([lambda e, c=c: e.matmul(pu[0:m, :], lhsT=wsrc[:, c, col0:col0 + m], rhs=hT[:, c, :],
                                                      start=(c == 0), stop=(c == 7)) for c in range(8)],
                             reads=[wsrc, hT], writes=[pu])
                        return pu

                    for j in range(5):
                        pu = inproj(j * 128, 128)
                        import os
                        if "D" not in os.environ.get("DBGSKIP", ""):
                            P.op("dve", lambda e: e.tensor_copy(out=uq_sb[:, j, :], in_=pu[:]), reads=[pu], writes=[uq_sb], acc=(j > 0))
                        if "A" not in os.environ.get("DBGSKIP", ""):
                            P.op("act", lambda e: e.activation(out=sq_sb[:, j, :], in_=pu[:], func=AF.Square), reads=[pu], writes=[sq_sb], acc=(j > 0))
                    if stop_after == "pA.lat1":
                        break
                    for (j0, nj, width, gT_) in ((0, 3, 384, gqT), (3, 2, 256, gkvT)):
                        pq = pq_r.next(); rs = rs_r.next()
                        P.mm([lambda e, j=j: e.matmul(pq[:], lhsT=ones_bf[:, 0:128], rhs=sq_sb[:, j0 + j, :],
                                                      start=(j == 0), stop=(j == nj - 1)) for j in range(nj)],
                             reads=[ones_bf, sq_sb], writes=[pq])
                        P.op("act", lambda e: e.activation(out=rs[:], in_=pq[:], func=AF.Sqrt, scale=1.0 / width, bias=eps_t[:, 0:1]),
                             reads=[pq, eps_t], writes=[rs])
                        P.op("dve", lambda e: e.reciprocal(out=rs[:], in_=rs[:]), reads=[rs], writes=[rs])
                        if stop_after == "pA.lat2":
                            break
                        for j in range(nj):
                            P.op("dve", lambda e, j=j: e.scalar_tensor_tensor(out=qn[:, j0 + j, :], in0=uq_sb[:, j0 + j, :],
                                                                             scalar=gT_[:, j:j + 1], in1=rs[:], op0=ALU.mult, op1=ALU.mult),
                                 reads=[uq_sb, rs, gT_], writes=[qn], acc=not (j0 == 0 and j == 0))
                    if stop_after == "pA.lat":
                        break
                    qT_sb = qT_sb_r.next()
                    for h in range(H):
                        pq = pq_r.next(); pqr = pq_r.next(); rt = rt_r.next()
                        P.mm([lambda e, c=c: e.matmul(pq[0:96, :], lhsT=wuq[:, c, h * 96:(h + 1) * 96], rhs=qn[:, c, :],
                                                      start=(c == 0), stop=(c == 2)) for c in range(3)],
                             reads=[wuq, qn], writes=[pq])
                        P.mm([lambda e, c=c: e.matmul(pqr[0:96, :], lhsT=wuqr[:, c, h * 96:(h + 1) * 96], rhs=qn[:, c, :],
                                                      start=(c == 0), stop=(c == 2)) for c in range(3)],
                             reads=[wuqr, qn], writes=[pqr])
                        P.op("act", lambda e: e.activation(out=qT_sb[0:64, h, :], in_=pq[0:64, :], func=AF.Copy, scale=96.0 ** -0.5),
                             reads=[pq], writes=[qT_sb], acc=(h > 0))
                        P.op("dve", lambda e: e.tensor_tensor(out=rt[64:96, 0, :], in0=pq[64:96, :], in1=tab[64:96, 0, :], op=ALU.mult),
                             reads=[pq, tab], writes=[rt])
                        P.op("dve", lambda e: e.tensor_tensor(out=rt[64:96, 1, :], in0=pqr[64:96, :], in1=tab[64:96, 1, :], op=ALU.mult),
                             reads=[pqr, tab], writes=[rt], acc=True)
                        P.op("dve", lambda e: e.tensor_tensor(out=qT_sb[64:96, h, :], in0=rt[64:96, 0, :], in1=rt[64:96, 1, :], op=ALU.add),
                             reads=[rt], writes=[qT_sb], acc=True)
                    P.dma("sp", lambda e: e.dma_start(out=qT_d[:, :, cols].rearrange("h r n -> r h n"), in_=qT_sb[:]), reads=[qT_sb])
                    if stop_after == "pA.q":
                        break
                    knT_sb = knT_sb_r.next()
                    for h in range(H):
                        pq = pq_r.next()
                        P.mm([lambda e, c=c: e.matmul(pq[0:64, :], lhsT=wukv[:, c, h * 128:h * 128 + 64], rhs=qn[:, 3 + c, :],
                                                      start=(c == 0), stop=(c == 1)) for c in range(2)],
                             reads=[wukv, qn], writes=[pq])
                        P.op("act", lambda e: e.activation(out=knT_sb[:, h, :], in_=pq[0:64, :], func=AF.Copy),
                             reads=[pq], writes=[knT_sb], acc=(h > 0))
                    P.dma("sp", lambda e: e.dma_start(out=knT_d[:, :, cols].rearrange("h r n -> r h n"), in_=knT_sb[:]), reads=[knT_sb])
                    if stop_after == "pA.kn":
                        break
                    krT_sb = krT_sb_r.next(); rt = rt_r.next()
                    pk = inproj(0, 96, lhs=wkr)
                    pkr = inproj(0, 96, lhs=wkrr)
                    P.op("dve", lambda e: e.tensor_tensor(out=rt[64:96, 0, :], in0=pk[64:96, :], in1=tab[64:96, 2, :], op=ALU.mult),
                         reads=[pk, tab], writes=[rt])
                    P.op("dve", lambda e: e.tensor_tensor(out=rt[64:96, 1, :], in0=pkr[64:96, :], in1=tab[64:96, 3, :], op=ALU.mult),
                         reads=[pkr, tab], writes=[rt], acc=True)
                    P.op("dve", lambda e: e.tensor_tensor(out=krT_sb[64:96, :], in0=rt[64:96, 0, :], in1=rt[64:96, 1, :], op=ALU.add),
                         reads=[rt], writes=[krT_sb])
                    P.dma("sp", lambda e: e.dma_start(out=krT_d[:, cols], in_=krT_sb[64:96, :]), reads=[krT_sb])
                    if stop_after == "pA.kr":
                        break
                    wv = wukv[:].rearrange("p c (h r) -> p c h r", h=8)
                    for ti in range(4):
                        t = g * 4 + ti
                        pu = pu_r.next(); v_sb = v_sb_r.next()
                        P.mm([lambda e, c=c: e.matmul(pu[:].rearrange("p (h r) -> p h r", h=8), lhsT=qn[:, 3 + c, ti * 128:(ti + 1) * 128],
                                                      rhs=wv[:, c, :, 64:128], start=(c == 0), stop=(c == 1)) for c in range(2)],
                             reads=[wukv, qn], writes=[pu])
                        P.op("act", lambda e: e.activation(out=v_sb[:], in_=pu[:], func=AF.Copy), reads=[pu], writes=[v_sb])
                        P.dma("sp", lambda e: e.dma_start(out=v_d[t * 128:(t + 1) * 128, :], in_=v_sb[:]), reads=[v_sb])
                        pu = pu_r.next(); f_sb = f_sb_r.next()
                        P.mm([lambda e, c=c: e.matmul(pu[:], lhsT=hT[:, c, ti * 128:(ti + 1) * 128], rhs=win[:, c, O3:O4],
                                                      start=(c == 0), stop=(c == 7)) for c in range(8)],
                             reads=[win, hT], writes=[pu])
                        P.op("dve", lambda e: e.tensor_copy(out=f_sb[:], in_=pu[:]), reads=[pu], writes=[f_sb])
                        P.dma("sp", lambda e: e.dma_start(out=uf_d[t * 128:(t + 1) * 128, :], in_=f_sb[:]), reads=[f_sb])
                    if stop_after == "pA.v":
                        break
                    for j in range(16):
                        pu = inproj(O4 + j * 128, 128)
                        g_sb = g_sb_r.next()
                        P.op("act", lambda e: e.activation(out=g_sb[:], in_=pu[:], func=AF.Sigmoid), reads=[pu], writes=[g_sb])
                        P.dma("sp", lambda e: e.dma_start(out=gT_d[j, :, cols], in_=g_sb[:]), reads=[g_sb])
                P.barrier()
            if stop_after == "pA":
                break

            with ExitStack() as ph:
                kT_r = Ring([sbuf(ph, "kT%d" % i, [96, T], BF16) for i in range(2)])
                qT_r = Ring([sbuf(ph, "qTb%d" % i, [96, T], BF16) for i in range(2)])
                vp_r = Ring([sbuf(ph, "vp%d" % i, [128, NT, 128], BF16) for i in range(2)])
                ps_r = Ring([psum(ph, "psS%d" % i, [128, 2, 512], F32) for i in range(3)])
                po_r = Ring([psum(ph, "psO%d" % i, [128, 512], F32) for i in range(2)])
                pt_r = Ring([sbuf(ph, "pt%d" % i, [128, 2, 512], BF16) for i in range(3)])
                r_r = Ring([sbuf(ph, "rr%d" % i, [64, 512], F32) for i in range(2)])
                o_r = Ring([sbuf(ph, "oo%d" % i, [64, 512], BF16) for i in range(2)])
                for vp in vp_r.b:
                    P.op("pool", lambda e: e.memset(vp[:], 1.0), writes=[vp])

                def load_head(h):
                    kT = kT_r.next(); qT = qT_r.next(); vp = vp_r.next()
                    P.dma("sp", lambda e: e.dma_start(out=kT[0:64, :], in_=knT_d[h, :, :]), writes=[kT])
                    P.dma("sp", lambda e: e.dma_start(out=kT[64:96, :], in_=krT_d[:, :]), writes=[kT], acc=True)
                    P.dma("sp", lambda e: e.dma_start(out=qT[:], in_=qT_d[h, :, :]), writes=[qT])
                    P.dma("sp", lambda e: e.dma_start(out=vp[:, :, 0:64],
                                                      in_=v_d[:, h * 64:(h + 1) * 64].rearrange("(t p) c -> p t c", p=128)),
                          writes=[vp], acc=True, deps=list(vp.r.items()) + list(vp.w.items()))
                    return kT, qT, vp

                NKP = NT // 2
                steps = [(h, qc, kp) for h in range(H) for qc in range(NG) for kp in range(NKP)]
                heads = {0: load_head(0)}
                st_ps = {}

                def emit_qk(i):
                    h, qc, kp = steps[i]
                    if h not in heads:
                        heads[h] = load_head(h)
                    kT, qT, vp = heads[h]
                    ps = ps_r.next()
                    P.mm([lambda e, u=u: e.matmul(ps[:, u, :], lhsT=kT[:, (2 * kp + u) * 128:(2 * kp + u + 1) * 128],
                                                  rhs=qT[:, qc * 512:(qc + 1) * 512], start=True, stop=True) for u in range(2)],
                         reads=[kT, qT], writes=[ps])
                    st_ps[i] = ps

                LOOK = 2
                for i in range(min(LOOK, len(steps))):
                    emit_qk(i)
                po = None
                for i, (h, qc, kp) in enumerate(steps):
                    if i + LOOK < len(steps):
                        emit_qk(i + LOOK)
                    kT, qT, vp = heads[h]
                    if kp == 0:
                        po = po_r.next()
                        if qc == 0 and h + 1 < H and (h + 1) not in heads:
                            heads[h + 1] = load_head(h + 1)
                    ps = st_ps.pop(i)
                    pt = pt_r.next()
                    mi = 2 * (kp // 16) + (qc // 8)
                    P.op("act", lambda e: e.activation(out=pt[:], in_=ps[:], func=AF.Exp, bias=mb[:, mi:mi + 1]),
                         reads=[ps, mb], writes=[pt])
                    P.mm([lambda e, u=u: e.matmul(po[:], lhsT=vp[:, 2 * kp + u, :], rhs=pt[:, u, :],
                                                  start=(kp == 0 and u == 0), stop=(kp == NKP - 1 and u == 1)) for u in range(2)],
                         reads=[vp, pt], writes=[po], acc=(kp > 0))
                    if kp == NKP - 1:
                        rr = r_r.next(); oo = o_r.next()
                        P.op("dve", lambda e: e.reciprocal(out=rr[:], in_=po[64:128, :]), reads=[po], writes=[rr])
                        P.op("dve", lambda e: e.tensor_tensor(out=oo[:], in0=po[0:64, :], in1=rr[:], op=ALU.mult),
                             reads=[po, rr], writes=[oo])
                        P.dma("sp", lambda e: e.dma_start(out=aoT_d[h * 64:(h + 1) * 64, qc * 512:(qc + 1) * 512], in_=oo[:]), reads=[oo])
                P.barrier()
            if stop_after == "pB":
                break

            with ExitStack() as ph:
                z = sbuf(ph, "z", [128, NT, 512], BF16)
                f_r = Ring([sbuf(ph, "fb%d" % i, [128, 2, 8 * 512], BF16) for i in range(3)])
                pacc = [psum(ph, "pacc%d" % i, [128, 512], F32) for i in range(8)]
                y_r = Ring([sbuf(ph, "ysb%d" % i, [128, 512], BF16) for i in range(4)])
                P.dma("sp", lambda e: e.dma_start(out=z[:], in_=uf_d[:, :].rearrange("(t p) c -> p t c", p=128)), writes=[z])
                for kc in range(NG):
                    for slab in range(8):
                        fb = f_r.next()
                        P.dma("sp", lambda e: e.dma_start(out=fb[:, 0, :], in_=fcs[0, kc, slab, :, :]), writes=[fb])
                        P.dma("sp", lambda e: e.dma_start(out=fb[:, 1, :], in_=fcs[1, kc, slab, :, :]), writes=[fb], acc=True)
                        fns = []
                        for j in range(8):
                            nt = slab * 8 + j
                            for cc in range(4):
                                for ri in range(2):
                                    fns.append(lambda e, nt=nt, j=j, cc=cc, ri=ri: e.matmul(
                                        pacc[ri * 4 + cc][:], lhsT=z[:, nt, cc * 128:(cc + 1) * 128],
                                        rhs=fb[:, ri, j * 512:(j + 1) * 512], start=(nt == 0), stop=(nt == NT - 1)))
                        P.mm(fns, reads=[z, fb], writes=pacc, acc=(slab > 0))
                    for i in range(8):
                        ri, cc = i // 4, i % 4
                        ysb = y_r.next()
                        if i % 2 == 0:
                            P.op("act", lambda e: e.activation(out=ysb[:], in_=pacc[i][:], func=AF.Copy), reads=[pacc[i]], writes=[ysb])
                        else:
                            P.op("dve", lambda e: e.tensor_copy(out=ysb[:], in_=pacc[i][:]), reads=[pacc[i]], writes=[ysb])
                        P.dma("sp", lambda e: e.dma_start(out=yfT_d[ri, cc * 128:(cc + 1) * 128, kc * 512:(kc + 1) * 512], in_=ysb[:]), reads=[ysb])
                P.barrier()
            if stop_after == "pC":
                break

            lay = ExitStack()
            lg_all = sbuf(lay, "lg_all", [128, NT, NE], F32)
            with ExitStack() as ph:
                wa = sbuf(ph, "wa_", [128, 4, D], BF16)
                wb = sbuf(ph, "wb_", [128, 4, D], BF16)
                wbc = sbuf(ph, "wbc", [128, 4, D], BF16)
                wbs = sbuf(ph, "wbs", [128, 4, D], BF16)
                wout = sbuf(ph, "wout", [128, 8, D], BF16)
                wr = sbuf(ph, "wr", [128, 8, NE], F32)
                brt = sbuf(ph, "brt", [128, NE], F32)
                bd = sbuf(ph, "bd", [128, 2, 128], BF16)
                ga1_b = [sbuf(ph, "ga1_b%d" % i, [128, D], F32) for i in range(2)]
                sh2_b = [sbuf(ph, "sh2_b%d" % i, [128, D], F32) for i in range(2)]
                gs2_b = [sbuf(ph, "gs2_b%d" % i, [128, D], F32) for i in range(2)]
                gff_b = sbuf(ph, "gff_b", [128, D], F32)
                pab_r = Ring([psum(ph, "pab%d" % i, [128, 512], F32) for i in range(3)])
                po_r = Ring([psum(ph, "pod%d" % i, [128, 512], F32) for i in range(2)])
                pT32 = psum(ph, "pT32", [128, 8, 128], F32)
                plg = psum(ph, "plg", [128, 512], F32)
                cast_load(wa, lambda c, cc, w: wa[:, c, cc:cc + w], w_a[l], D, 4)
                cast_load(wb, lambda c, cc, w: wb[:, c, cc:cc + w], w_b[l], D, 4)
                cast_load(wout, lambda c, cc, w: wout[:, c, cc:cc + w], w_out[l], D, 8)
                P.dma("sp", lambda e: e.dma_start(out=wr[:], in_=w_router[l, :, :].rearrange("(c p) n -> p c n", p=128)), writes=[wr])
                P.dma("sp", lambda e: e.dma_start(out=brt[:], in_=b_router[l:l + 1, :].partition_broadcast(128)), writes=[brt])
                P.dma("sp", lambda e: e.dma_start(out=bd[:], in_=bd_d[:, :, :].rearrange("i p n -> p i n")), writes=[bd])
                P.dma("sp", lambda e: e.dma_start(out=gff_b[:], in_=g_ffn[l:l + 1, :].partition_broadcast(128)), writes=[gff_b])
                for s_ in range(2):
                    P.dma("sp", lambda e: e.dma_start(out=ga1_b[s_][:], in_=modd[s_:s_ + 1, 2 * D:3 * D].partition_broadcast(128)), writes=[ga1_b[s_]])
                    P.dma("sp", lambda e: e.dma_start(out=sh2_b[s_][:], in_=modd[s_:s_ + 1, 3 * D:4 * D].partition_broadcast(128)), writes=[sh2_b[s_]])
                    P.dma("sp", lambda e: e.dma_start(out=gs2_b[s_][:], in_=modd[s_:s_ + 1, 4 * D:5 * D].partition_broadcast(128)), writes=[gs2_b[s_]])
                    P.op("dve", lambda e: e.scalar_tensor_tensor(out=gs2_b[s_][:], in0=gs2_b[s_][:], scalar=1.0, in1=gff_b[:],
                                                               op0=ALU.add, op1=ALU.mult), reads=[gs2_b[s_], gff_b], writes=[gs2_b[s_]])
                for i, dst in enumerate((wbc, wbs)):
                    for c in range(4):
                        for half in range(2):
                            po = po_r.next()
                            P.mm([lambda e: e.matmul(po[:], lhsT=bd[:, i, :], rhs=wb[:, c, half * 512:(half + 1) * 512], start=True, stop=True)],
                                 reads=[bd, wb], writes=[po])
                            P.op("act", lambda e: e.activation(out=dst[:, c, half * 512:(half + 1) * 512], in_=po[:], func=AF.Copy),
                                 reads=[po], writes=[dst], acc=not (c == 0 and half == 0))
                ao_r = Ring([sbuf(ph, "ao%d" % i, [128, 4, 512], BF16) for i in range(2)])
                yr_r = Ring([sbuf(ph, "yr%d" % i, [128, 2, 4, 512], BF16) for i in range(2)])
                gg_r = Ring([sbuf(ph, "gg%d" % i, [128, 16, 512], BF16) for i in range(2)])
                t1_r = Ring([sbuf(ph, "t1_%d" % i, [128, 512], F32) for i in range(2)])
                t2_r = Ring([sbuf(ph, "t2_%d" % i, [128, 512], F32) for i in range(2)])
                mg_r = Ring([sbuf(ph, "mg%d" % i, [128, 8, 512], BF16) for i in range(2)])
                xd_r = Ring([sbuf(ph, "xd%d" % i, [128, D], F32) for i in range(2)])
                x1_r = Ring([sbuf(ph, "x1_%d" % i, [128, D], F32) for i in range(2)])
                h2f_r = Ring([sbuf(ph, "h2f%d" % i, [128, D], F32) for i in range(2)])
                h2b_r = Ring([sbuf(ph, "h2b%d" % i, [128, D], BF16) for i in range(2)])
                h2T_r = Ring([sbuf(ph, "h2T%d" % i, [128, 8, 128], F32) for i in range(2)])
                sqd = sbuf(ph, "sqd", [128, D], BF16)
                ssd_r = Ring([sbuf(ph, "ssd%d" % i, [128, 2], F32) for i in range(2)])

                def load_group(g):
                    cols = slice(g * 512, (g + 1) * 512)
                    ao = ao_r.next(); yr = yr_r.next(); gg = gg_r.next()
                    P.dma("sp", lambda e: e.dma_start(out=ao[:], in_=aoT_d[:, cols].rearrange("(c p) n -> p c n", p=128)), writes=[ao])
                    for ri in range(2):
                        P.dma("sp", lambda e: e.dma_start(out=yr[:, ri, :, :], in_=yfT_d[ri, :, cols].rearrange("(c p) n -> p c n", p=128)),
                              writes=[yr], acc=(ri > 0))
                    P.dma("sp", lambda e: e.dma_start(out=gg[:], in_=gT_d[:, :, cols].rearrange("j p n -> p j n")), writes=[gg])
                    return ao, yr, gg

                nxt = load_group(0)
                for g in range(NG):
                    slot = g // 8
                    ao, yr, gg = nxt
                    if g + 1 < NG:
                        nxt = load_group(g + 1)
                    mg = mg_r.next()
                    for dc in range(8):
                        dcs = slice(dc * 128, (dc + 1) * 128)
                        pa = pab_r.next()
                        P.mm([lambda e, c=c: e.matmul(pa[:], lhsT=wa[:, c, dcs], rhs=ao[:, c, :], start=(c == 0), stop=(c == 3))
                              for c in range(4)], reads=[wa, ao], writes=[pa])
                        pb = pab_r.next()
                        P.mm([lambda e, c=c: e.matmul(pb[:], lhsT=(wbc if c < 4 else wbs)[:, c % 4, dcs], rhs=yr[:, c // 4, c % 4, :],
                                                      start=(c == 0), stop=(c == 7)) for c in range(8)], reads=[wbc, wbs, yr], writes=[pb])
                        t1 = t1_r.next(); t2 = t2_r.next()
                        P.op("dve", lambda e: e.tensor_tensor(out=t1[:], in0=pa[:], in1=gg[:, dc, :], op=ALU.mult), reads=[pa, gg], writes=[t1])
                        P.op("dve", lambda e: e.tensor_tensor(out=t2[:], in0=pb[:], in1=gg[:, 8 + dc, :], op=ALU.mult), reads=[pb, gg], writes=[t2])
                        P.op("pool", lambda e: e.tensor_tensor(out=mg[:, dc, :], in0=t1[:], in1=t2[:], op=ALU.add),
                             reads=[t1, t2], writes=[mg], acc=(dc > 0))
                    for ti in range(4):
                        t = g * 4 + ti
                        rows = slice(t * 128, (t + 1) * 128)
                        xd = xd_r.next(); x1 = x1_r.next()
                        P.dma("sp", lambda e: e.dma_start(out=xd[:], in_=x_src[rows, :]), writes=[xd])
                        for half in range(2):
                            hs_ = slice(half * 512, (half + 1) * 512)
                            po = po_r.next()
                            P.mm([lambda e, dc=dc: e.matmul(po[:], lhsT=mg[:, dc, ti * 128:(ti + 1) * 128], rhs=wout[:, dc, hs_],
                                                            start=(dc == 0), stop=(dc == 7)) for dc in range(8)], reads=[mg, wout], writes=[po])
                            P.op("dve", lambda e: e.tensor_tensor(out=x1[:, hs_], in0=po[:], in1=ga1_b[slot][:, hs_], op=ALU.mult),
                                 reads=[po, ga1_b[slot]], writes=[x1], acc=(half > 0))
                        P.op("pool", lambda e: e.tensor_tensor(out=x1[:], in0=x1[:], in1=xd[:], op=ALU.add), reads=[x1, xd], writes=[x1])
                        P.dma("sp", lambda e: e.dma_start(out=xmid[rows, :], in_=x1[:]), reads=[x1])
                        ss = ssd_r.next(); h2f = h2f_r.next(); h2b = h2b_r.next(); h2T = h2T_r.next()
                        P.op("act", lambda e: e.activation(out=sqd[:], in_=x1[:], func=AF.Square, accum_out=ss[:, 0:1]), reads=[x1], writes=[sqd, ss])
                        P.op("act", lambda e: e.activation(out=ss[:, 1:2], in_=ss[:, 0:1], func=AF.Sqrt, scale=1.0 / D, bias=eps_t[:, 0:1]),
                             reads=[ss, eps_t], writes=[ss], acc=True)
                        P.op("dve", lambda e: e.reciprocal(out=ss[:, 1:2], in_=ss[:, 1:2]), reads=[ss], writes=[ss], acc=True)
                        P.op("dve", lambda e: e.scalar_tensor_tensor(out=h2f[:], in0=x1[:], scalar=ss[:, 1:2], in1=gs2_b[slot][:],
                                                                   op0=ALU.mult, op1=ALU.mult), reads=[x1, ss, gs2_b[slot]], writes=[h2f])
                        P.op("pool", lambda e: e.tensor_tensor(out=h2f[:], in0=h2f[:], in1=sh2_b[slot][:], op=ALU.add),
                             reads=[h2f, sh2_b[slot]], writes=[h2f])
                        P.op("act", lambda e: e.activation(out=h2b[:], in_=h2f[:], func=AF.Copy), reads=[h2f], writes=[h2b])
                        P.dma("sp", lambda e: e.dma_start(out=h2_d[rows, :], in_=h2b[:]), reads=[h2b])
                        P.mm([lambda e, c=c: e.transpose(out=pT32[:, c, :], in_=h2f[:, c * 128:(c + 1) * 128], identity=ident_f[:])
                              for c in range(8)], reads=[h2f, ident_f], writes=[pT32])
                        P.op("act", lambda e: e.activation(out=h2T[:], in_=pT32[:], func=AF.Copy), reads=[pT32], writes=[h2T])
                        P.mm([lambda e, c=c: e.matmul(plg[:, 0:NE], lhsT=h2T[:, c, :], rhs=wr[:, c, :], start=(c == 0), stop=(c == 7))
                              for c in range(8)], reads=[h2T, wr], writes=[plg])
                        P.op("dve", lambda e: e.tensor_tensor(out=lg_all[:, t, :], in0=plg[:, 0:NE], in1=brt[:], op=ALU.add),
                             reads=[plg, brt], writes=[lg_all], acc=(t > 0))
                if "logits" in dbg:
                    lgd = dscr("logits", [T, NE], F32)
                    P.dma("sp", lambda e: e.dma_start(out=lgd[:, :].rearrange("(t p) n -> p t n", p=128), in_=lg_all[:]), reads=[lg_all])
                P.barrier()
            if stop_after == "pD":
                lay.close()
                break

            idx4 = sbuf(lay, "idx4", [128, NT, 4], I32)
            g4 = sbuf(lay, "g4", [128, NT, 4], F32)
            widx = sbuf(lay, "widx", [128, NBLK, 8], I32)
            bidx = sbuf(lay, "bidx", [128, NBLK], I32)
            with ExitStack() as ph:
                m8 = sbuf(ph, "m8", [128, NT, 8], F32)
                mask = sbuf(ph, "mask", [128, NT, NE], BF16)
                gsum = sbuf(ph, "gsum", [128, NT], F32)
                cb = sbuf(ph, "cb", [128, NT, NE], F32)
                sa = sbuf(ph, "sa", [128, NT, NE], F32)
                sb_ = sbuf(ph, "sb_", [128, NT, NE], F32)
                slot = sbuf(ph, "slot", [128, NT, NE], F32)
                oh = sbuf(ph, "oh", [128, NT, NE], F32)
                slot4 = sbuf(ph, "slot4", [128, NT, 4], F32)
                ntot = sbuf(ph, "ntot", [128, NE], F32)
                pad = sbuf(ph, "pad", [128, NE], F32)
                pend = sbuf(ph, "pend", [128, NE], F32)
                pstart = sbuf(ph, "pstart", [128, NE], F32)
                ones32 = sbuf(ph, "ones32", [128, NE], F32)
                jj_i = sbuf(ph, "jj_i", [128, NBLK], I32)
                jj = sbuf(ph, "jj", [128, NBLK], F32)
                cmp = sbuf(ph, "cmp", [128, NBLK, NE], F32)
                cmp2 = sbuf(ph, "cmp2", [128, NE, 16], F32)
                ej = sbuf(ph, "ej", [128, NBLK], F32)
                pc_i = sbuf(ph, "pc_i", [128, 8], I32)
                pc = sbuf(ph, "pc", [128, 8], F32)
                wf = sbuf(ph, "wf", [128, NBLK, 8], F32)
                pcnt = [psum(ph, "pcnt%d" % i, [128, 512], F32) for i in range(4)]
                ppos = [psum(ph, "ppos%d" % i, [128, 512], F32) for i in range(4)]
                for t in range(NT):
                    P.op("dve", lambda e: e.max(out=m8[:, t, :], in_=lg_all[:, t, :]), reads=[lg_all], writes=[m8], acc=(t > 0))
                P.op("dve", lambda e: e.tensor_tensor(out=mask[:], in0=lg_all[:], in1=m8[:, :, 3:4].to_broadcast([128, NT, NE]), op=ALU.is_ge),
                     reads=[lg_all, m8], writes=[mask])
                P.op("dve", lambda e: e.tensor_tensor(out=g4[:], in0=m8[:, :, 0:4], in1=m8[:, :, 0:1].to_broadcast([128, NT, 4]), op=ALU.subtract),
                     reads=[m8], writes=[g4])
                P.op("act", lambda e: e.activation(out=g4[:], in_=g4[:], func=AF.Exp), reads=[g4], writes=[g4])
                P.op("dve", lambda e: e.tensor_reduce(out=gsum[:], in_=g4[:], axis=AX.X, op=ALU.add), reads=[g4], writes=[gsum])
                P.op("dve", lambda e: e.reciprocal(out=gsum[:], in_=gsum[:]), reads=[gsum], writes=[gsum])
                P.op("dve", lambda e: e.tensor_tensor(out=g4[:], in0=g4[:], in1=gsum[:].unsqueeze(2).to_broadcast([128, NT, 4]), op=ALU.mult),
                     reads=[g4, gsum], writes=[g4])
                mflat = mask[:].rearrange("p t e -> p (t e)")
                for q in range(4):
                    P.mm([lambda e: e.matmul(pcnt[q][:], lhsT=ones_bf[:, 0:128], rhs=mflat[:, q * 512:(q + 1) * 512], start=True, stop=True)],
                         reads=[ones_bf, mask], writes=[pcnt[q]])
                    P.mm([lambda e: e.matmul(ppos[q][:], lhsT=ltri[:], rhs=mflat[:, q * 512:(q + 1) * 512], start=True, stop=True)],
                         reads=[ltri, mask], writes=[ppos[q]])
                    P.op("act", lambda e: e.activation(out=cb[:].rearrange("p t e -> p (t e)")[:, q * 512:(q + 1) * 512], in_=pcnt[q][:], func=AF.Copy),
                         reads=[pcnt[q]], writes=[cb], acc=(q > 0))
                P.op("dve", lambda e: e.tensor_copy(out=sa[:], in_=cb[:]), reads=[cb], writes=[sa])
                a, b = sa, sb_
                sh = 1
                while sh < NT:
                    P.op("dve", lambda e: e.tensor_copy(out=b[:, 0:sh, :], in_=a[:, 0:sh, :]), reads=[a], writes=[b])
                    P.op("dve", lambda e: e.tensor_tensor(out=b[:, sh:NT, :], in0=a[:, sh:NT, :], in1=a[:, 0:NT - sh, :], op=ALU.add),
                         reads=[a], writes=[b], acc=True)
                    a, b = b, a
                    sh *= 2
                incl = a
                P.op("dve", lambda e: e.tensor_copy(out=ntot[:], in_=incl[:, NT - 1, :]), reads=[incl], writes=[ntot])
                P.op("pool", lambda e: e.iota(jj_i[:], pattern=[[512, NBLK]], base=0, channel_multiplier=0), writes=[jj_i])
                P.op("pool", lambda e: e.iota(pc_i[:], pattern=[[128, 8]], base=l * NE * D, channel_multiplier=1), writes=[pc_i])
                P.op("dve", lambda e: e.tensor_copy(out=jj[:], in_=jj_i[:]), reads=[jj_i], writes=[jj])
                P.op("dve", lambda e: e.tensor_copy(out=pc[:], in_=pc_i[:]), reads=[pc_i], writes=[pc])
                P.op("dve", lambda e: e.tensor_tensor(out=cmp2[:], in0=ntot[:].unsqueeze(2).to_broadcast([128, NE, 16]),
                                                     in1=jj[:, 0:16].unsqueeze(1).to_broadcast([128, NE, 16]), op=ALU.is_gt),
                     reads=[ntot, jj], writes=[cmp2])
                P.op("dve", lambda e: e.tensor_reduce(out=pad[:], in_=cmp2[:], axis=AX.X, op=ALU.add), reads=[cmp2], writes=[pad])
                P.op("dve", lambda e: e.tensor_scalar(out=pad[:], in0=pad[:], scalar1=512.0, scalar2=None, op0=ALU.mult), reads=[pad], writes=[pad])
                P.op("dve", lambda e: e.memset(ones32[:], 1.0), writes=[ones32])
                P.op("dve", lambda e: e.tensor_tensor_scan(out=pend[:], data0=ones32[:], data1=pad[:], initial=0.0, op0=ALU.mult, op1=ALU.add),
                     reads=[ones32, pad], writes=[pend])
                P.op("dve", lambda e: e.tensor_tensor(out=pstart[:], in0=pend[:], in1=pad[:], op=ALU.subtract), reads=[pend, pad], writes=[pstart])
                P.op("dve", lambda e: e.tensor_tensor(out=b[:], in0=incl[:], in1=cb[:], op=ALU.subtract), reads=[incl, cb], writes=[b])
                P.op("dve", lambda e: e.tensor_tensor(out=b[:], in0=b[:], in1=pstart[:].unsqueeze(1).to_broadcast([128, NT, NE]), op=ALU.add),
                     reads=[b, pstart], writes=[b])
                bflat = b[:].rearrange("p t e -> p (t e)")
                sflat = slot[:].rearrange("p t e -> p (t e)")
                for q in range(4):
                    P.op("dve", lambda e: e.tensor_tensor(out=sflat[:, q * 512:(q + 1) * 512], in0=ppos[q][:], in1=bflat[:, q * 512:(q + 1) * 512], op=ALU.add),
                         reads=[ppos[q], b], writes=[slot], acc=(q > 0))
                for k in range(4):
                    P.op("dve", lambda e: e.tensor_tensor(out=oh[:], in0=lg_all[:], in1=m8[:, :, k:k + 1].to_broadcast([128, NT, NE]), op=ALU.is_equal),
                         reads=[lg_all, m8], writes=[oh])
                    P.op("dve", lambda e: e.tensor_tensor(out=oh[:], in0=oh[:], in1=slot[:], op=ALU.mult), reads=[oh, slot], writes=[oh])
                    P.op("dve", lambda e: e.tensor_reduce(out=slot4[:, :, k], in_=oh[:], axis=AX.X, op=ALU.add), reads=[oh], writes=[slot4], acc=(k > 0))
                P.op("dve", lambda e: e.tensor_copy(out=idx4[:], in_=slot4[:]), reads=[slot4], writes=[idx4])
                P.op("dve", lambda e: e.tensor_tensor(out=cmp[:], in0=pend[:].unsqueeze(1).to_broadcast([128, NBLK, NE]),
                                                     in1=jj[:].unsqueeze(2).to_broadcast([128, NBLK, NE]), op=ALU.is_le),
                     reads=[pend, jj], writes=[cmp])
                P.op("dve", lambda e: e.tensor_reduce(out=ej[:], in_=cmp[:], axis=AX.X, op=ALU.add), reads=[cmp], writes=[ej])
                P.op("dve", lambda e: e.tensor_scalar(out=ej[:], in0=ej[:], scalar1=float(NE - 1), scalar2=None, op0=ALU.min), reads=[ej], writes=[ej])
                P.op("dve", lambda e: e.scalar_tensor_tensor(out=wf[:], in0=ej[:].unsqueeze(2).to_broadcast([128, NBLK, 8]), scalar=float(D),
                                                           in1=pc[:].unsqueeze(1).to_broadcast([128, NBLK, 8]), op0=ALU.mult, op1=ALU.add),
                     reads=[ej, pc], writes=[wf])
                P.op("dve", lambda e: e.tensor_copy(out=widx[:], in_=wf[:]), reads=[wf], writes=[widx])
                P.op("dve", lambda e: e.tensor_scalar(out=ej[:], in0=ej[:], scalar1=float(l * NE), scalar2=None, op0=ALU.add), reads=[ej], writes=[ej])
                P.op("dve", lambda e: e.tensor_copy(out=bidx[:], in_=ej[:]), reads=[ej], writes=[bidx])
                if "idx4" in dbg:
                    for nm, src, shp, dt_ in (("idx4", idx4, [128, NT * 4], I32), ("g4", g4, [128, NT * 4], F32),
                                              ("widx", widx, [128, NBLK * 8], I32), ("bidx", bidx, [128, NBLK], I32)):
                        dd = dscr(nm, shp, dt_)
                        flat = src[:] if len(src.t.shape) == 2 else src[:].rearrange("p a b -> p (a b)")
                        P.dma("sp", lambda e: e.dma_start(out=dd[:, :], in_=flat), reads=[src])
                P.barrier()
            if stop_after == "pE1":
                lay.close()
                break

            with ExitStack() as ph:
                hb_r = Ring([sbuf(ph, "hb%d" % i, [128, D], BF16) for i in range(4)])
                for t in range(NT):
                    hb = hb_r.next()
                    P.dma("sp", lambda e: e.dma_start(out=hb[:], in_=h2_d[t * 128:(t + 1) * 128, :]), writes=[hb])
                    for k in range(4):
                        P.dma("pool", lambda e: e.indirect_dma_start(
                            out=hs_d[:, :], out_offset=bass.IndirectOffsetOnAxis(ap=idx4[:, t, k:k + 1], axis=0),
                            in_=hb[:], in_offset=None), reads=[hb, idx4], sembuf=hb)
                P.barrier()
            if stop_after == "pE2":
                lay.close()
                break

            with ExitStack() as ph:
                wg_r = Ring([sbuf(ph, "wg%d" % i, [128, 8, 2 * D], BF16) for i in range(2)])
                wd_r = Ring([sbuf(ph, "wd%d" % i, [128, 8, D], BF16) for i in range(2)])
                bg_r = Ring([sbuf(ph, "bg%d" % i, [128, 2 * D], BF16) for i in range(2)])
                bdn_r = Ring([sbuf(ph, "bdn%d" % i, [128, D], BF16) for i in range(2)])
                hsT_r = Ring([sbuf(ph, "hsT%d" % i, [128, 8, BLK], BF16) for i in range(2)])
                actT_r = Ring([sbuf(ph, "actT%d" % i, [128, 8, BLK], BF16) for i in range(2)])
                sig_r = Ring([sbuf(ph, "sig%d" % i, [128, BLK], F32) for i in range(2)])
                t1_r = Ring([sbuf(ph, "et1_%d" % i, [128, BLK], F32) for i in range(2)])
                uc_r = Ring([sbuf(ph, "uc%d" % i, [128, BLK], F32) for i in range(2)])
                yo_r = Ring([sbuf(ph, "yo%d" % i, [128, D], BF16) for i in range(3)])
                pg_r = Ring([psum(ph, "pg%d" % i, [128, 512], F32) for i in range(2)])
                pu_r = Ring([psum(ph, "pue%d" % i, [128, 512], F32) for i in range(2)])
                po_r = Ring([psum(ph, "poe%d" % i, [128, 512], F32) for i in range(3)])

                def load_block(j):
                    wg = wg_r.next(); wd = wd_r.next(); bg = bg_r.next(); bdn = bdn_r.next(); hsT = hsT_r.next()
                    for c in range(8):
                        P.dma("sp", lambda e: e.dma_start_transpose(out=hsT[:, c, :], in_=hs_d[j * BLK:(j + 1) * BLK, c * 128:(c + 1) * 128]),
                              writes=[hsT], acc=(c > 0))
                    for c in range(8):
                        P.dma("pool", lambda e: e.indirect_dma_start(
                            out=wg[:, c, :], out_offset=None, in_=w_gu[:, :],
                            in_offset=bass.IndirectOffsetOnAxis(ap=widx[:, j, c:c + 1], axis=0)), reads=[widx], writes=[wg], acc=(c > 0))
                    P.dma("pool", lambda e: e.indirect_dma_start(
                        out=bg[:], out_offset=None, in_=b_gu[:, :],
                        in_offset=bass.IndirectOffsetOnAxis(ap=bidx[:, j:j + 1], axis=0)), reads=[bidx], writes=[bg])
                    for c in range(8):
                        P.dma("pool", lambda e: e.indirect_dma_start(
                            out=wd[:, c, :], out_offset=None, in_=w_dn[:, :],
                            in_offset=bass.IndirectOffsetOnAxis(ap=widx[:, j, c:c + 1], axis=0)), reads=[widx], writes=[wd], acc=(c > 0))
                    P.dma("pool", lambda e: e.indirect_dma_start(
                        out=bdn[:], out_offset=None, in_=b_dn[:, :],
                        in_offset=bass.IndirectOffsetOnAxis(ap=bidx[:, j:j + 1], axis=0)), reads=[bidx], writes=[bdn])
                    return wg, wd, bg, bdn, hsT

                nblk_run = NBLK
                nxt = load_block(0)
                for j in range(nblk_run):
                    wg, wd, bg, bdn, hsT = nxt
                    if j + 1 < nblk_run:
                        nxt = load_block(j + 1)
                    actT = actT_r.next()
                    for fo in range(8):
                        pg = pg_r.next(); pu = pu_r.next()
                        gsl = slice(fo * 256, (fo + 1) * 256, 2)
                        usl = slice(fo * 256 + 1, (fo + 1) * 256, 2)
                        P.mm([lambda e, c=c: e.matmul(pg[:], lhsT=wg[:, c, gsl], rhs=hsT[:, c, :], start=(c == 0), stop=False) for c in range(8)]
                             + [lambda e: e.matmul(pg[:], lhsT=bg[0:1, gsl], rhs=ones_bf[0:1, 0:BLK], start=False, stop=True)],
                             reads=[wg, hsT, bg, ones_bf], writes=[pg])
                        P.mm([lambda e, c=c: e.matmul(pu[:], lhsT=wg[:, c, usl], rhs=hsT[:, c, :], start=(c == 0), stop=False) for c in range(8)]
                             + [lambda e: e.matmul(pu[:], lhsT=bg[0:1, usl], rhs=ones_bf[0:1, 0:BLK], start=False, stop=True)],
                             reads=[wg, hsT, bg, ones_bf], writes=[pu])
                        sig = sig_r.next(); t1 = t1_r.next(); uc = uc_r.next()
                        P.op("act", lambda e: e.activation(out=sig[:], in_=pg[:], func=AF.Sigmoid, scale=1.702), reads=[pg], writes=[sig])
                        P.op("dve", lambda e: e.scalar_tensor_tensor(out=t1[:], in0=pg[:], scalar=7.0, in1=sig[:], op0=ALU.min, op1=ALU.mult),
                             reads=[pg, sig], writes=[t1])
                        P.op("dve", lambda e: e.tensor_scalar(out=uc[:], in0=pu[:], scalar1=-7.0, scalar2=7.0, op0=ALU.max, op1=ALU.min),
                             reads=[pu], writes=[uc])
                        P.op("dve", lambda e: e.scalar_tensor_tensor(out=actT[:, fo, :], in0=uc[:], scalar=1.0, in1=t1[:], op0=ALU.add, op1=ALU.mult),
                             reads=[uc, t1], writes=[actT], acc=(fo > 0))
                    for ts in range(4):
                        yo = yo_r.next()
                        for half in range(2):
                            hsl = slice(half * 512, (half + 1) * 512)
                            po = po_r.next()
                            P.mm([lambda e, fo=fo: e.matmul(po[:], lhsT=actT[:, fo, ts * 128:(ts + 1) * 128], rhs=wd[:, fo, hsl],
                                                            start=(fo == 0), stop=False) for fo in range(8)]
                                 + [lambda e: e.matmul(po[:], lhsT=ones_bf[0:1, 0:128], rhs=bdn[0:1, hsl], start=False, stop=True)],
                                 reads=[actT, wd, bdn, ones_bf], writes=[po])
                            if half == 0:
                                P.op("act", lambda e: e.activation(out=yo[:, hsl], in_=po[:], func=AF.Copy), reads=[po], writes=[yo])
                            else:
                                P.op("dve", lambda e: e.tensor_copy(out=yo[:, hsl], in_=po[:]), reads=[po], writes=[yo], acc=True)
                        P.dma("sp", lambda e: e.dma_start(out=ys_d[j * BLK + ts * 128:j * BLK + (ts + 1) * 128, :], in_=yo[:]), reads=[yo])
                P.barrier()
            if stop_after == "pE3":
                lay.close()
                break

            with ExitStack() as ph:
                ga2_b = [sbuf(ph, "ga2_b%d" % i, [128, D], F32) for i in range(2)]
                gfin_b = sbuf(ph, "gfin_b", [128, D], F32)
                for s_ in range(2):
                    P.dma("sp", lambda e: e.dma_start(out=ga2_b[s_][:], in_=modd[s_:s_ + 1, 5 * D:6 * D].partition_broadcast(128)), writes=[ga2_b[s_]])
                P.dma("sp", lambda e: e.dma_start(out=gfin_b[:], in_=g_final[0:1, :].partition_broadcast(128)), writes=[gfin_b])
                xm_r = Ring([sbuf(ph, "xm%d" % i, [128, D], F32) for i in range(3)])
                yg_r = Ring([sbuf(ph, "yg%d" % i, [128, 4, D], BF16) for i in range(3)])
                ac_r = Ring([sbuf(ph, "ac%d" % i, [128, D], F32) for i in range(2)])
                x2_r = Ring([sbuf(ph, "x2_%d" % i, [128, D], F32) for i in range(3)])
                sqf = sbuf(ph, "sqf", [128, D], BF16)
                ssf_r = Ring([sbuf(ph, "ssf%d" % i, [128, 2], F32) for i in range(2)])
                x_dst = y_out if last else xres

                def load_tile(t):
                    xm = xm_r.next(); yg = yg_r.next()
                    P.dma("sp", lambda e: e.dma_start(out=xm[:], in_=xmid[t * 128:(t + 1) * 128, :]), writes=[xm])
                    for k in range(4):
                        P.dma("pool", lambda e: e.indirect_dma_start(
                            out=yg[:, k, :], out_offset=None, in_=ys_d[:, :],
                            in_offset=bass.IndirectOffsetOnAxis(ap=idx4[:, t, k:k + 1], axis=0)), reads=[idx4], writes=[yg], acc=(k > 0))
                    return xm, yg

                nxt = load_tile(0)
                for t in range(NT):
                    slot_ = t // 32
                    xm, yg = nxt
                    if t + 1 < NT:
                        nxt = load_tile(t + 1)
                    ac = ac_r.next(); x2 = x2_r.next()
                    P.op("dve", lambda e: e.tensor_scalar(out=ac[:], in0=yg[:, 0, :], scalar1=g4[:, t, 0:1], scalar2=None, op0=ALU.mult),
                         reads=[yg, g4], writes=[ac])
                    for k in range(1, 4):
                        P.op("dve", lambda e: e.scalar_tensor_tensor(out=ac[:], in0=yg[:, k, :], scalar=g4[:, t, k:k + 1], in1=ac[:],
                                                                   op0=ALU.mult, op1=ALU.add), reads=[yg, g4, ac], writes=[ac])
                    P.op("pool", lambda e: e.tensor_tensor(out=ac[:], in0=ac[:], in1=ga2_b[slot_][:], op=ALU.mult), reads=[ac, ga2_b[slot_]], writes=[ac])
                    P.op("pool", lambda e: e.tensor_tensor(out=x2[:], in0=ac[:], in1=xm[:], op=ALU.add), reads=[ac, xm], writes=[x2])
                    if last:
                        ss = ssf_r.next()
                        P.op("act", lambda e: e.activation(out=sqf[:], in_=x2[:], func=AF.Square, accum_out=ss[:, 0:1]), reads=[x2], writes=[sqf, ss])
                        P.op("act", lambda e: e.activation(out=ss[:, 1:2], in_=ss[:, 0:1], func=AF.Sqrt, scale=1.0 / D, bias=eps_t[:, 0:1]),
                             reads=[ss, eps_t], writes=[ss], acc=True)
                        P.op("dve", lambda e: e.reciprocal(out=ss[:, 1:2], in_=ss[:, 1:2]), reads=[ss], writes=[ss], acc=True)
                        P.op("dve", lambda e: e.scalar_tensor_tensor(out=x2[:], in0=x2[:], scalar=ss[:, 1:2], in1=gfin_b[:],
                                                                   op0=ALU.mult, op1=ALU.mult), reads=[x2, ss, gfin_b], writes=[x2])
                    P.dma("sp", lambda e: e.dma_start(out=x_dst[t * 128:(t + 1) * 128, :], in_=x2[:]), reads=[x2])
                P.barrier()
            lay.close()
        P.barrier()
    return nc


_CONST_CACHE = {}


def _consts(kind):
    if kind in _CONST_CACHE:
        return _CONST_CACHE[kind]
    S = 4096 if kind == "prompt" else 8192
    bf16 = ml_dtypes.bfloat16
    pos = (np.arange(T) % S).astype(np.float32)
    inv = (1.0 / (10000.0 ** (np.arange(0, 32, 2, dtype=np.float32) / 32.0))).astype(np.float32)
    ang = pos[:, None] * inv[None, :]
    cos = np.cos(ang).astype(np.float32).T
    sin = np.sin(ang).astype(np.float32).T
    c2 = np.concatenate([cos, cos], 0)
    s2 = np.concatenate([sin, sin], 0)
    sc = np.float32(96.0 ** -0.5)
    rope = np.stack([c2 * sc, s2 * sc, c2, s2], 0).astype(np.float32)
    maskb = np.zeros((128, 4), np.float32)
    if kind == "prompt":
        maskb[:, 1] = -30000.0
        maskb[:, 2] = -30000.0
    n = np.arange(T, dtype=np.int64)
    fcs = np.empty((2, NG, 8, 128, 8 * 512), dtype=bf16)
    scale = 1.0 / np.sqrt(S * 64.0)
    seq_n = n // S
    for kc in range(NG):
        k = np.arange(kc * 512, (kc + 1) * 512, dtype=np.int64)
        prod = ((n % S)[:, None] * (k % S)[None, :]) % S
        angk = prod.astype(np.float64) * (2.0 * np.pi / S)
        same = (seq_n[:, None] == (k // S)[None, :])
        fc = np.where(same, np.cos(angk) * scale, 0.0).astype(np.float32)
        fs = np.where(same, np.sin(angk) * scale, 0.0).astype(np.float32)
        for i, f in enumerate((fc, fs)):
            f = f.reshape(8, 8, 128, 512).transpose(0, 2, 1, 3).reshape(8, 128, 8 * 512)
            fcs[i, kc] = f.astype(bf16)
    cj = np.arange(64)
    a64 = 2.0 * np.pi * np.outer(cj, cj) / 64.0
    bd = np.zeros((2, 128, 128), np.float32)
    for b in range(2):
        bd[0, b * 64:(b + 1) * 64, b * 64:(b + 1) * 64] = np.cos(a64)
        bd[1, b * 64:(b + 1) * 64, b * 64:(b + 1) * 64] = -np.sin(a64)
    out = dict(rope=rope, maskb=maskb, fcs=fcs, bd=bd.astype(bf16),
               ident_bf=np.eye(128, dtype=np.float32).astype(bf16), ident_f=np.eye(128, dtype=np.float32),
               ltri=np.triu(np.ones((128, 128), np.float32), 1).astype(bf16))
    _CONST_CACHE[kind] = out
    return out


def make_in_maps(inputs, depth, cores=range(8)):
    f = lambda a: np.ascontiguousarray(np.asarray(a))
    xp, xs = f(inputs["x_prompt"]), f(inputs["x_sample"])
    cp, cs = f(inputs["c_prompt"]), f(inputs["c_sample"])
    shared = {}
    for k in ("w_ada", "b_ada", "g_mix", "g_ffn", "w_in", "g_q", "w_uq", "g_kv", "w_ukv", "w_a", "w_b", "w_out",
              "w_router", "b_router"):
        shared[k] = f(inputs[k])[:depth]
    shared["w_gu"] = f(inputs["w_gu"])[:depth].reshape(depth * NE * D, 2 * D)
    shared["b_gu"] = f(inputs["b_gu"])[:depth].reshape(depth * NE, 2 * D)
    shared["w_dn"] = f(inputs["w_dn"])[:depth].reshape(depth * NE * D, D)
    shared["b_dn"] = f(inputs["b_dn"])[:depth].reshape(depth * NE, D)
    shared["g_final"] = f(inputs["g_final"]).reshape(1, D)
    maps = []
    for c in cores:
        m = dict(shared)
        if c < 4:
            m["x"] = xp[2 * c:2 * c + 2].reshape(T, D)
            m["c2"] = cp[2 * c:2 * c + 2]
            m.update(_consts("prompt"))
        else:
            m["x"] = xs[c - 4].reshape(T, D)
            m["c2"] = np.stack([cs[c - 4], cs[c - 4]], 0)
            m.update(_consts("sample"))
        maps.append(m)
    return maps


_NC_CACHE = {}


def kernel(**inputs):
    depth = 4
    if depth not in _NC_CACHE:
        _NC_CACHE[depth] = build_program(depth)
    nc = _NC_CACHE[depth]
    maps = make_in_maps(inputs, depth)
    res = run_bass_kernel_spmd(nc, maps, core_ids=list(range(8)))
    ys = [np.asarray(r["y"], dtype=np.float32) for r in res.results]
    y_prompt = np.stack([ys[c].reshape(2, 4096, D) for c in range(4)], 0).reshape(8, 4096, D)
    y_sample = np.stack([ys[c].reshape(T, D) for c in range(4, 8)], 0)
    return (y_prompt, y_sample)
```
